# Optimizing a Trainium2 kernel written in Bass

```python
import math
import jax, jax.numpy as jnp
from jax import lax
import numpy as np

D_MODEL = 1024
BATCH = 8
SEQ = 2048
DEPTH = 2

CHUNK = 64
Q_BLOCK = 128
NORM_EPS = 1e-6
RWKV_HEADS = 8
RWKV_HEAD_DIM = 64
RWKV_WIDTH = RWKV_HEADS * RWKV_HEAD_DIM
DECAY_LORA = 64
ICLR_LORA = 64
GATE_LORA = 128
DECAY_SCALE = math.exp(-0.5)
LNX_EPS = 64e-5
MLA_HEADS = 8
Q_LORA = 384
KV_LORA = 256
QK_NOPE = 64
QK_ROPE = 32
V_HEAD = 64
ROPE_THETA = 10000.0
D_FF = 2816
N_EXPERTS = 8
TOP_K = 2
D_FF_EXPERT = 3584
N_DENSE = (DEPTH + 1) // 2
N_MOE = DEPTH // 2
SHIFT_SIZES = (RWKV_WIDTH, RWKV_WIDTH, RWKV_WIDTH, DECAY_LORA, ICLR_LORA, GATE_LORA)
REST_SIZES = (Q_LORA, KV_LORA, QK_ROPE, D_MODEL, D_MODEL)
SHIFT_WIDTH = sum(SHIFT_SIZES)
IN_WIDTH = SHIFT_WIDTH + sum(REST_SIZES)

kernel_name = 'hybrid_rwkv7_mla_moe_adaln_block'


def _split(x, sizes):
    out, o = [], 0
    for s in sizes:
        out.append(x[..., o:o + s])
        o += s
    return out


def rmsnorm(x, g):
    xf = x.astype(jnp.float32)
    y = xf * lax.rsqrt(jnp.mean(xf * xf, axis=-1, keepdims=True) + NORM_EPS)
    return (y * g.astype(jnp.float32)).astype(x.dtype)


def ada_modulation(c, w, b):
    mod = jax.nn.silu(c) @ w + b
    shift, scale, gate = jnp.split(mod, 3, axis=-1)
    return shift[:, None, :], scale[:, None, :], gate[:, None, :]


def token_shift(p):
    return jnp.pad(p, ((0, 0), (1, 0), (0, 0)))[:, :-1]


def apply_rope(x, cos, sin):
    x1, x2 = jnp.split(x, 2, axis=-1)
    return jnp.concatenate([x1 * cos - x2 * sin, x1 * sin + x2 * cos], axis=-1)


def _rwkv7_step(state, inp):
    r, w, k, v, a, b = inp
    sa = jnp.einsum('bhvk,bhk->bhv', state, a)
    state = state * w[:, :, None, :] + sa[..., None] * b[:, :, None, :] + v[..., None] * k[:, :, None, :]
    y = jnp.einsum('bhvk,bhk->bhv', state, r)
    return state, y


def rwkv7_branch(p_r, p_k, p_v, p_w, p_a, p_g, w0, w_up, a0, a_up, g_up, k_k, k_a, r_k, lnx_g, lnx_b, w_out):
    dt = p_r.dtype
    B, S, _ = p_r.shape
    H, N = RWKV_HEADS, RWKV_HEAD_DIM
    f = lambda t: t.astype(jnp.float32)
    decay = jnp.exp(-DECAY_SCALE * jax.nn.sigmoid(f(w0) + jnp.tanh(f(p_w)) @ f(w_up)))
    iclr = jax.nn.sigmoid(f(a0) + f(p_a) @ f(a_up))
    gate = jax.nn.sigmoid(f(p_g)) @ f(g_up)
    k = f(p_k)
    kk = (k * f(k_k)).reshape(B, S, H, N)
    kk = kk * lax.rsqrt(jnp.sum(kk * kk, axis=-1, keepdims=True) + 1e-12)
    k = k * (1.0 + (iclr - 1.0) * f(k_a))
    heads = lambda t: t.reshape(B, S, H, N)
    r, k, v, decay, iclr = heads(f(p_r)), heads(k), heads(f(p_v)), heads(decay), heads(iclr)
    a_vec = -kk
    b_vec = kk * iclr
    xs = tuple(jnp.swapaxes(t, 0, 1) for t in (r, decay, k, v, a_vec, b_vec))
    state0 = jnp.zeros((B, H, N, N), jnp.float32)
    _, y = lax.scan(_rwkv7_step, state0, xs)
    y = jnp.swapaxes(y, 0, 1)
    mu = jnp.mean(y, axis=-1, keepdims=True)
    var = jnp.mean(jnp.square(y - mu), axis=-1, keepdims=True)
    y = ((y - mu) * lax.rsqrt(var + LNX_EPS)).reshape(B, S, H * N) * f(lnx_g) + f(lnx_b)
    bonus = jnp.sum(r * k * f(r_k), axis=-1, keepdims=True) * v
    y = (y + bonus.reshape(B, S, H * N)) * gate
    return y.astype(dt) @ w_out


def mla_branch(q_a, kv_a, k_rope, cos, sin, q_norm, w_qb, kv_norm, w_kvb, w_out):
    B, S, _ = q_a.shape
    H = MLA_HEADS
    q = (rmsnorm(q_a, q_norm) @ w_qb).reshape(B, S, H, QK_NOPE + QK_ROPE)
    q_nope, q_pe = q[..., :QK_NOPE], q[..., QK_NOPE:]
    q_pe = apply_rope(q_pe, cos[:, :, None, :], sin[:, :, None, :])
    kv = (rmsnorm(kv_a, kv_norm) @ w_kvb).reshape(B, S, H, QK_NOPE + V_HEAD)
    k_nope, v = kv[..., :QK_NOPE], kv[..., QK_NOPE:]
    k_pe = apply_rope(k_rope, cos, sin)
    q = jnp.concatenate([q_nope, q_pe], axis=-1)
    k = jnp.concatenate([k_nope, jnp.broadcast_to(k_pe[:, :, None, :], (B, S, H, QK_ROPE))], axis=-1)
    scale = (QK_NOPE + QK_ROPE) ** -0.5
    outs = []
    for i in range(S // Q_BLOCK):
        q_lo = i * Q_BLOCK
        k_hi = q_lo + Q_BLOCK
        s = jnp.einsum('bqhd,bkhd->bhqk', q[:, q_lo:k_hi], k[:, :k_hi]).astype(jnp.float32) * scale
        q_chunk = (q_lo + jnp.arange(Q_BLOCK)) // CHUNK
        k_chunk = jnp.arange(k_hi) // CHUNK
        mask = k_chunk[None, :] <= q_chunk[:, None]
        s = jnp.where(mask, s, jnp.finfo(jnp.float32).min)
        p = jax.nn.softmax(s, axis=-1).astype(v.dtype)
        outs.append(jnp.einsum('bhqk,bkhd->bqhd', p, v[:, :k_hi]))
    o = jnp.concatenate(outs, axis=1).reshape(B, S, H * V_HEAD)
    return o @ w_out


def swiglu(h, w1, w3, w2):
    return (jax.nn.silu(h @ w1) * (h @ w3)) @ w2


def moe_swiglu(h, router_w, router_b, w1, w3, w2):
    logits = (h @ router_w).astype(jnp.float32) + router_b.astype(jnp.float32)
    top_v, top_i = lax.top_k(logits, TOP_K)
    probs = jax.nn.softmax(top_v, axis=-1)
    combine = jnp.sum(jax.nn.one_hot(top_i, N_EXPERTS, dtype=jnp.float32) * probs[..., None], axis=-2)
    out = jnp.zeros_like(h)
    for e in range(N_EXPERTS):
        out = out + combine[..., e:e + 1].astype(h.dtype) * swiglu(h, w1[e], w3[e], w2[e])
    return out


def setup_inputs(seed: int = 0) -> dict:
    key = jax.random.key(seed)
    keys = iter(jax.random.split(key, 48))
    def nrm(shape, s):
        return s * jax.random.normal(next(keys), shape, jnp.float32)
    L = DEPTH
    RW = RWKV_WIDTH
    x = nrm((BATCH, SEQ, D_MODEL), 1.0)
    c = nrm((BATCH, D_MODEL), 1.0)
    offs = jax.random.randint(next(keys), (BATCH, 1), 0, 4096, dtype=jnp.int32)
    positions = offs + jnp.arange(SEQ, dtype=jnp.int32)[None, :]
    return {
        'x': x,
        'c': c,
        'positions': positions,
        'ada_w': nrm((L, 2, D_MODEL, 3 * D_MODEL), 0.5 * D_MODEL ** -0.5),
        'ada_b': nrm((L, 2, 3 * D_MODEL), 0.02),
        'norm_mix': 1.0 + nrm((L, D_MODEL), 0.05),
        'norm_ffn': 1.0 + nrm((L, D_MODEL), 0.05),
        'norm_final': 1.0 + nrm((D_MODEL,), 0.05),
        'w_in': nrm((L, D_MODEL, IN_WIDTH), D_MODEL ** -0.5),
        'tshift_mu': jax.random.uniform(next(keys), (L, SHIFT_WIDTH), jnp.float32),
        'w0': nrm((L, RW), 0.5),
        'w_up': nrm((L, DECAY_LORA, RW), 0.5 * DECAY_LORA ** -0.5),
        'a0': nrm((L, RW), 0.1),
        'a_up': nrm((L, ICLR_LORA, RW), 0.5 * ICLR_LORA ** -0.5),
        'g_up': nrm((L, GATE_LORA, RW), GATE_LORA ** -0.5),
        'k_k': 0.85 + nrm((L, RW), 0.05),
        'k_a': 1.0 + nrm((L, RW), 0.05),
        'r_k': nrm((L, RWKV_HEADS, RWKV_HEAD_DIM), 0.1),
        'lnx_g': 1.0 + nrm((L, RW), 0.05),
        'lnx_b': nrm((L, RW), 0.02),
        'rwkv_out': nrm((L, RW, D_MODEL), RW ** -0.5),
        'q_norm': 1.0 + nrm((L, Q_LORA), 0.05),
        'w_qb': nrm((L, Q_LORA, MLA_HEADS * (QK_NOPE + QK_ROPE)), Q_LORA ** -0.5),
        'kv_norm': 1.0 + nrm((L, KV_LORA), 0.05),
        'w_kvb': nrm((L, KV_LORA, MLA_HEADS * (QK_NOPE + V_HEAD)), KV_LORA ** -0.5),
        'mla_out': nrm((L, MLA_HEADS * V_HEAD, D_MODEL), (MLA_HEADS * V_HEAD) ** -0.5),
        'w_o': nrm((L, D_MODEL, D_MODEL), D_MODEL ** -0.5),
        'ffn_w1': nrm((N_DENSE, D_MODEL, D_FF), D_MODEL ** -0.5),
        'ffn_w3': nrm((N_DENSE, D_MODEL, D_FF), D_MODEL ** -0.5),
        'ffn_w2': nrm((N_DENSE, D_FF, D_MODEL), D_FF ** -0.5),
        'router_w': nrm((N_MOE, D_MODEL, N_EXPERTS), D_MODEL ** -0.5),
        'router_b': nrm((N_MOE, N_EXPERTS), 0.01),
        'moe_w1': nrm((N_MOE, N_EXPERTS, D_MODEL, D_FF_EXPERT), D_MODEL ** -0.5),
        'moe_w3': nrm((N_MOE, N_EXPERTS, D_MODEL, D_FF_EXPERT), D_MODEL ** -0.5),
        'moe_w2': nrm((N_MOE, N_EXPERTS, D_FF_EXPERT, D_MODEL), D_FF_EXPERT ** -0.5),
    }


def reference(x, c, positions, ada_w, ada_b, norm_mix, norm_ffn, norm_final, w_in, tshift_mu,
              w0, w_up, a0, a_up, g_up, k_k, k_a, r_k, lnx_g, lnx_b, rwkv_out,
              q_norm, w_qb, kv_norm, w_kvb, mla_out, w_o,
              ffn_w1, ffn_w3, ffn_w2, router_w, router_b, moe_w1, moe_w3, moe_w2):
    inv_freq = ROPE_THETA ** (-jnp.arange(0, QK_ROPE, 2, dtype=jnp.float32) / QK_ROPE)
    ang = positions.astype(jnp.float32)[..., None] * inv_freq
    cos = jnp.cos(ang).astype(x.dtype)
    sin = jnp.sin(ang).astype(x.dtype)
    for l in range(DEPTH):
        shift, scale, gate = ada_modulation(c, ada_w[l, 0], ada_b[l, 0])
        h = rmsnorm(x, norm_mix[l]) * (1.0 + scale) + shift
        proj = h @ w_in[l]
        ps = proj[..., :SHIFT_WIDTH]
        ps = ps + (token_shift(ps) - ps) * tshift_mu[l]
        p_r, p_k, p_v, p_w, p_a, p_g = _split(ps, SHIFT_SIZES)
        q_a, kv_a, k_rope, g_a, g_b = _split(proj[..., SHIFT_WIDTH:], REST_SIZES)
        y_a = rwkv7_branch(p_r, p_k, p_v, p_w, p_a, p_g, w0[l], w_up[l], a0[l], a_up[l], g_up[l],
                           k_k[l], k_a[l], r_k[l], lnx_g[l], lnx_b[l], rwkv_out[l])
        y_b = mla_branch(q_a, kv_a, k_rope, cos, sin, q_norm[l], w_qb[l], kv_norm[l], w_kvb[l], mla_out[l])
        merged = jax.nn.sigmoid(g_a) * y_a + jax.nn.sigmoid(g_b) * y_b
        x = x + gate * (merged @ w_o[l])
        shift, scale, gate = ada_modulation(c, ada_w[l, 1], ada_b[l, 1])
        h = rmsnorm(x, norm_ffn[l]) * (1.0 + scale) + shift
        if l % 2 == 0:
            f = swiglu(h, ffn_w1[l // 2], ffn_w3[l // 2], ffn_w2[l // 2])
        else:
            f = moe_swiglu(h, router_w[l // 2], router_b[l // 2], moe_w1[l // 2], moe_w3[l // 2], moe_w2[l // 2])
        x = x + gate * f
    return rmsnorm(x, norm_final)
```

```python
import math
import numpy as np
import concourse.bass as bass
import concourse.mybir as mybir
from concourse.bass_utils import run_bass_kernel_spmd
from contextlib import ExitStack

F32 = mybir.dt.float32
BF16 = mybir.dt.bfloat16
I32 = mybir.dt.int32
ALU = mybir.AluOpType
AF = mybir.ActivationFunctionType
AX = mybir.AxisListType

S = 2048
D = 1024
NL = 2
NCH = 36
DECAY_SCALE = math.exp(-0.5)
LNX_EPS = 64e-5
NORM_EPS = 1e-6
NF_DENSE = 22
NF_MOE = 28
NEXP = 8

VEC_COLS = {}
_off = 0


def _vc(name, n):
    global _off
    VEC_COLS[name] = (_off, n)
    _off += n


for _l in range(NL):
    _vc(f"norm_mix{_l}", 8)
    _vc(f"norm_ffn{_l}", 8)
    _vc(f"ada_b{_l}0", 24)
    _vc(f"ada_b{_l}1", 24)
    _vc(f"mu{_l}", 14)
    _vc(f"q_norm{_l}", 3)
    _vc(f"kv_norm{_l}", 2)
    for _n in ("w0", "a0", "k_k", "k_a", "r_k", "lnx_g", "lnx_b"):
        _vc(f"{_n}{_l}", 8)
_vc("norm_final", 8)
_vc("ropef", 1)
_vc("ropes", 1)
_vc("router_b", 8)
NV = _off


class StopBuild(Exception):
    pass


class Tok:
    __slots__ = ("w", "r")

    def __init__(self):
        self.w = None
        self.r = {}


def toks(n):
    return [Tok() for _ in range(n)]


class Ctx:
    def __init__(self, nc, es):
        self.nc = nc
        self.eng = {"pe": nc.tensor, "act": nc.scalar, "dve": nc.vector, "pool": nc.gpsimd, "sp": nc.sync}
        self.NR = 8
        self.counters = ["pe", "act", "dve", "pool"] + [f"{q}_d{j}" for q in ("sp", "act", "pool") for j in range(self.NR)]
        self.dma_idx = {"sp": 0, "act": 0, "pool": 0}
        self.sem = {c: es.enter_context(nc.semaphore("s_" + c)) for c in self.counters}
        self.cnt = {c: 0 for c in self.counters}
        self.seen = {s: {c: 0 for c in self.counters} for s in self.eng}

    def _wait(self, stream, deps):
        for c, v in deps.items():
            if c == "pe" and stream == "pe":
                continue
            if self.seen[stream][c] < v:
                self.eng[stream].wait_ge(self.sem[c], v)
                self.seen[stream][c] = v

    def _deps(self, counter, r, w):
        deps = {}
        for t in r:
            if t.w is not None and t.w[1] > deps.get(t.w[0], 0):
                deps[t.w[0]] = t.w[1]
        for t in w:
            if t.w is not None and t.w[1] > deps.get(t.w[0], 0):
                deps[t.w[0]] = t.w[1]
            for c, v in t.r.items():
                if c == counter and "_d" not in c:
                    continue
                if v > deps.get(c, 0):
                    deps[c] = v
        return deps

    def _done(self, counter, v, r, w):
        for t in r:
            if t.r.get(counter, 0) < v:
                t.r[counter] = v
        for t in w:
            t.w = (counter, v)
            t.r = {}

    def op(self, stream, fn, r=(), w=()):
        self._wait(stream, self._deps(stream, r, w))
        inst = fn(self.eng[stream])
        self.cnt[stream] += 1
        v = self.cnt[stream]
        inst.then_inc(self.sem[stream], 1)
        self._done(stream, v, r, w)

    def dma(self, stream, out, in_, r=(), w=()):
        counter = f"{stream}_d{self.dma_idx[stream] % self.NR}"
        self.dma_idx[stream] += 1
        if self.cnt[counter] > 0:
            self._wait(stream, {counter: self.cnt[counter]})
        self._wait(stream, self._deps(counter, r, w))
        inst = self.eng[stream].dma_start(out=out, in_=in_)
        self.cnt[counter] += 16
        v = self.cnt[counter]
        inst.then_inc(self.sem[counter], 16)
        self._done(counter, v, r, w)

    def barrier(self):
        for s in self.eng:
            self._wait(s, {c: self.cnt[c] for c in self.counters if self.cnt[c] > 0 and c != s})


def mm(cx, out, lhsT, rhs, start, stop, r=(), w=()):
    cx.op("pe", lambda e: e.matmul(out, lhsT=lhsT, rhs=rhs, start=start, stop=stop), r=r, w=w)


def act(cx, out, in_, func, r=(), w=(), bias=None, scale=None):
    kw = {}
    if bias is not None:
        kw["bias"] = bias
    if scale is not None:
        kw["scale"] = scale
    cx.op("act", lambda e: e.activation(out=out, in_=in_, func=func, **kw), r=r, w=w)


def tt(cx, eng, out, in0, in1, op, r=(), w=()):
    cx.op(eng, lambda e: e.tensor_tensor(out=out, in0=in0, in1=in1, op=op), r=r, w=w)


def ts(cx, eng, out, in0, s1, s2, op0, op1=None, r=(), w=()):
    if op1 is None:
        cx.op(eng, lambda e: e.tensor_scalar(out=out, in0=in0, scalar1=s1, scalar2=None, op0=op0), r=r, w=w)
    else:
        cx.op(eng, lambda e: e.tensor_scalar(out=out, in0=in0, scalar1=s1, scalar2=s2, op0=op0, op1=op1), r=r, w=w)


def stt(cx, out, in0, scalar, in1, op0, op1, r=(), w=()):
    cx.op("dve", lambda e: e.scalar_tensor_tensor(out=out, in0=in0, scalar=scalar, in1=in1, op0=op0, op1=op1), r=r, w=w)


def cp(cx, eng, out, in_, r=(), w=()):
    if eng == "act":
        cx.op("act", lambda e: e.activation(out=out, in_=in_, func=AF.Copy), r=r, w=w)
    else:
        cx.op(eng, lambda e: e.tensor_copy(out=out, in_=in_), r=r, w=w)


def bc3(ap, n):
    p, h = ap.shape
    return ap.unsqueeze(2).to_broadcast([p, h, n])


def build(stop="all", dbg=None):
    nc = bass.Bass("TRN2", target_bir_lowering=False)

    def din(name, shape, dt=F32):
        return nc.dram_tensor(name, list(shape), dt, kind="ExternalInput").ap()

    def dscr(name, shape, dt=F32):
        kind = "ExternalOutput" if (dbg and name in dbg) else "Internal"
        return nc.dram_tensor(name, list(shape), dt, kind=kind).ap()

    xT_in = din("xT_in", [D, S])
    cT_in = din("cT", [128, 8])
    pos_in = din("pos", [32, S], I32)
    vecs_in = din("vecs", [128, NV])
    consts_in = din("consts", [128, 5, 128])
    c64_in = din("c64", [64, 2, 8, 128])
    sel_in = din("sel", [16, 8, 128])
    adaw_in = din("adaw", [NL, 2, 128, 8, 3072])
    win_in = din("win", [NL, NCH // 4, 128, 4, 8, 128])
    waup_in = din("waup", [NL, 128, 512])
    gup_in = din("gup", [NL, 128, 512])
    rout_in = din("rout", [NL, 64, 8, 1024])
    wqb_in = din("wqb", [NL, 128, 3, 768])
    wqbs_in = din("wqbs", [NL, 128, 3, 768])
    wkvk_in = din("wkvk", [NL, 128, 2, 512])
    wkvv_in = din("wkvv", [NL, 128, 2, 512])
    mout_in = din("mout", [NL, 64, 8, 1024])
    wo_in = din("wo", [NL, 128, 8, 1024])
    ffn_in = din("ffnp", [NF_DENSE, 128, 3072])
    moe_in = din("moep", [NEXP, NF_MOE, 128, 3072])
    rw_in = din("routw", [128, 8, 8])
    out_T = nc.dram_tensor("outT", [D, S], F32, kind="ExternalOutput").ap()

    projT = [dscr(f"projT{l}", [NCH * 128, S]) for l in range(NL)]
    yaT_d = [dscr(f"yaT{l}", [512, S], BF16) for l in range(NL)]
    oT_d = [dscr(f"oT{l}", [512, S], BF16) for l in range(NL)]
    xs_d = dscr("xs", [D, S])
    rope_d = dscr("rope", [2, 32, S])

    es = ExitStack()
    try:
        with es:
            cx = Ctx(nc, es)

            uid = [0]

            def sb(name, shape, dt=F32, scope=None):
                uid[0] += 1
                return (scope or es).enter_context(nc.sbuf_tensor(f"sb{uid[0]}_{name}", list(shape), dt))

            def ps(name, shape, dt=F32, scope=None):
                uid[0] += 1
                return (scope or es).enter_context(nc.psum_tensor(f"ps{uid[0]}_{name}", list(shape), dt))

            taps = {}

            def tap(name, ap, rtoks):
                stop_here = dbg is not None and (name + "!") in dbg
                if not dbg or (name not in dbg and not stop_here):
                    return
                shp = list(ap.shape)
                d = nc.dram_tensor(name, shp, F32, kind="ExternalOutput").ap()
                tk = Tok()
                cx.dma("sp", d, ap, r=rtoks, w=[tk])
                cx.barrier()
                if stop_here:
                    raise StopBuild()

            class Stager:
                def __init__(self, scope, ncols, nbuf=2, parts=128):
                    self.bufs = [sb("stg", [parts, ncols], F32, scope=scope) for _ in range(nbuf)]
                    self.tk = toks(nbuf)
                    self.i = 0

                def load(self, dst2d, src2d, eng, w, r=()):
                    p, n = src2d.shape
                    b = self.i % len(self.bufs)
                    self.i += 1
                    cx.dma("sp", self.bufs[b][0:p, 0:n], src2d, r=list(r), w=[self.tk[b]])
                    cp(cx, eng, dst2d, self.bufs[b][0:p, 0:n], r=[self.tk[b]], w=w)

            vecs = sb("vecs", [128, NV]); t_vecs = Tok()
            consts = sb("consts", [128, 5, 128]); t_consts = Tok()
            c64 = sb("c64", [64, 2, 8, 128]); t_c64 = Tok()
            sel = sb("sel", [16, 8, 128]); t_sel = Tok()
            onesb = sb("onesb", [128, 128], BF16); t_onesb = Tok()
            identb = sb("identb", [128, 128], BF16)
            ones64 = sb("ones64", [64, 256]); t_ones64 = Tok()
            mods = sb("mods", [128, NL * 2, 24]); t_mods = Tok()
            acol = sb("acol", [128, NL * 2 + 1, 8]); t_acol = Tok()
            omm = sb("omm", [128, NL, 14]); t_omm = Tok()
            omka = sb("omka", [64, NL, 8]); t_omka = Tok()
            sc = sb("sc", [128, 8]); t_sc = Tok()

            cx.dma("sp", vecs[:], vecs_in[:, :], w=[t_vecs])
            cx.dma("sp", consts[:], consts_in[:, :, :], w=[t_consts])
            cx.dma("sp", c64[:], c64_in[:, :, :, :], w=[t_c64])
            cx.dma("sp", sel[:], sel_in[:, :, :], w=[t_sel])
            cx.dma("sp", sc[:], cT_in[:, :], w=[t_sc])
            ident = consts[:, 0, :]
            ones128 = consts[:, 1, :]
            mlamask = consts[:, 2, :]
            cp(cx, "dve", onesb[:], consts[:, 1, :], r=[t_consts], w=[t_onesb])
            cp(cx, "dve", identb[:], consts[:, 0, :], r=[t_consts], w=[t_onesb])
            cx.op("dve", lambda e: e.memset(ones64[:], 1.0), w=[t_ones64])

            def V(name, rows=128):
                o, n = VEC_COLS[name]
                return vecs[0:rows, o:o + n]

            act(cx, sc[:], sc[:], AF.Silu, r=[t_sc], w=[t_sc])

            with ExitStack() as ph:
                wb = [sb(f"adawb{i}", [128, 8, 512], scope=ph) for i in range(2)]
                t_wb = toks(2)
                pmod = ps("pmod", [128, 512], scope=ph); t_pmod = Tok()
                it = 0
                for l in range(NL):
                    for j in range(2):
                        for g in range(6):
                            b = it % 2
                            it += 1
                            cx.dma("sp", wb[b][:], adaw_in[l, j, :, :, g * 512:(g + 1) * 512], w=[t_wb[b]])
                            for nn in range(4):
                                col = g * 4 + nn
                                for kc in range(8):
                                    mm(cx, pmod[:, col:col + 1], wb[b][:, kc, nn * 128:(nn + 1) * 128], sc[:, kc:kc + 1],
                                       kc == 0, kc == 7, r=[t_wb[b], t_sc], w=[t_pmod])
                        o, n = VEC_COLS[f"ada_b{l}{j}"]
                        tt(cx, "dve", mods[:, l * 2 + j, :], pmod[:, 0:24], vecs[:, o:o + 24], ALU.add,
                           r=[t_pmod, t_vecs], w=[t_mods])
                        nm = V(f"norm_mix{l}") if j == 0 else V(f"norm_ffn{l}")
                        stt(cx, acol[:, l * 2 + j, :], mods[:, l * 2 + j, 8:16], 1.0, nm, ALU.add, ALU.mult,
                            r=[t_mods, t_vecs], w=[t_acol])
                cp(cx, "dve", acol[:, NL * 2, :], V("norm_final"), r=[t_vecs], w=[t_acol])
                for l in range(NL):
                    ts(cx, "dve", omm[:, l, :], V(f"mu{l}"), -1.0, 1.0, ALU.mult, ALU.add, r=[t_vecs], w=[t_omm])
                    ts(cx, "dve", omka[:, l, :], V(f"k_a{l}", 64), -1.0, 1.0, ALU.mult, ALU.add, r=[t_vecs], w=[t_omka])
                cx.barrier()
            t_small = [t_vecs, t_consts, t_c64, t_sel, t_onesb, t_ones64, t_mods, t_acol, t_omm, t_omka]

            def rms_to_h(ph, xT, t_x, hT, t_h, a_ap, shift_ap, h32=None, t_h32=None, tb_hook=None):
                sq = [sb(f"sq{i}", [128, 512], scope=ph) for i in range(2)]; t_sq = toks(2)
                rstd = [sb(f"rstd{i}", [128, 512], scope=ph) for i in range(2)]; t_rstd = toks(2)
                tmp = [sb(f"ntmp{i}", [128, 512], scope=ph) for i in range(2)]; t_tmp = toks(2)
                pss = [ps(f"pss{i}", [128, 512], scope=ph) for i in range(2)]; t_pss = toks(2)
                k = 0
                for tb in range(4):
                    cs = slice(tb * 512, (tb + 1) * 512)
                    pb = tb % 2
                    for kc in range(8):
                        b = k % 2
                        k += 1
                        act(cx, sq[b][:], xT[:, kc, cs], AF.Square, r=[t_x[kc]], w=[t_sq[b]])
                        mm(cx, pss[pb][:], ones128, sq[b][:], kc == 0, kc == 7, r=[t_sq[b], t_consts], w=[t_pss[pb]])
                    act(cx, rstd[pb][:], pss[pb][:], AF.Sqrt, r=[t_pss[pb]], w=[t_rstd[pb]], bias=NORM_EPS, scale=1.0 / D)
                    cx.op("dve", lambda e: e.reciprocal(out=rstd[pb][:], in_=rstd[pb][:]), r=[t_rstd[pb]], w=[t_rstd[pb]])
                    for kc in range(8):
                        b = k % 2
                        k += 1
                        tt(cx, "dve", tmp[b][:], xT[:, kc, cs], rstd[pb][:], ALU.mult, r=[t_x[kc], t_rstd[pb]], w=[t_tmp[b]])
                        if h32 is not None:
                            ts(cx, "pool", h32[:, kc, :], tmp[b][:], a_ap[:, kc:kc + 1], shift_ap[:, kc:kc + 1], ALU.mult, ALU.add,
                               r=[t_tmp[b]] + t_small, w=[t_h32])
                            cp(cx, "act", hT[:, kc, 1 + tb * 512:1 + (tb + 1) * 512], h32[:, kc, :], r=[t_h32], w=[t_h[kc]])
                        else:
                            act(cx, hT[:, kc, 1 + tb * 512:1 + (tb + 1) * 512], tmp[b][:], AF.Identity,
                                r=[t_tmp[b]] + t_small, w=[t_h[kc]], bias=shift_ap[:, kc:kc + 1], scale=a_ap[:, kc:kc + 1])
                    if tb_hook is not None:
                        tb_hook(tb)

            xT = None
            xscope = None
            t_x = toks(8)
            t_rope = Tok()
            for l in range(NL):
                if xT is None:
                    xscope = ExitStack()
                    xT = sb("xT", [128, 8, S], scope=xscope)
                    for kc in range(8):
                        cx.dma("sp", xT[:, kc, :], xT_in[kc * 128:(kc + 1) * 128, :], w=[t_x[kc]])
                with ExitStack() as ph:
                    hT = sb("hT", [128, 8, S + 1], BF16, scope=ph); t_h = toks(8)
                    for kc in range(8):
                        cx.op("pool", lambda e: e.memset(hT[:, kc, 0:1], 0.0), w=[t_h[kc]])
                    with ExitStack() as ph2:
                        rms_to_h(ph2, xT, t_x, hT, t_h, acol[:, l * 2, :], mods[:, l * 2, 0:8])
                        cx.barrier()
                    t_xs = Tok()
                    for kc in range(8):
                        cx.dma("sp", xs_d[kc * 128:(kc + 1) * 128, :], xT[:, kc, :], r=[t_x[kc]], w=[t_xs])
                    with ExitStack() as ph2:
                        wbuf = [sb(f"wbuf{i}", [128, 32, 128], BF16, scope=ph2) for i in range(2)]; t_wbuf = toks(2)
                        stg1 = Stager(ph2, 4096, 2)
                        pp = [ps(f"pp{i}", [128, S], scope=ph2) for i in range(2)]; t_pp = toks(2)
                        Pst = [sb(f"Pst{i}", [128, S + 1], scope=ph2) for i in range(2)]; t_P = toks(2)
                        Tst_ = sb("Ptmp", [128, S], scope=ph2); t_T = Tok()
                        Ost = [sb(f"Ost{i}", [128, S], scope=ph2) for i in range(2)]; t_O = toks(2)
                        for i in range(2):
                            cx.op("pool", lambda e: e.memset(Pst[i][:, 0:1], 0.0), w=[t_P[i]])
                        t_proj = Tok()
                        for g in range(NCH // 4):
                            wbi = g % 2
                            stg1.load(wbuf[wbi][:].rearrange("p a m -> p (a m)"), win_in[l, g, :, :, :, :].rearrange("p c k m -> p (c k m)"),
                                      "dve" if g % 2 == 0 else "act", [t_wbuf[wbi]])
                            for ci in range(4):
                                m = g * 4 + ci
                                pi = m % 2
                                for tb in range(4):
                                    for kc in range(8):
                                        mm(cx, pp[pi][:, tb * 512:(tb + 1) * 512], wbuf[wbi][:, ci * 8 + kc, :],
                                           hT[:, kc, 1 + tb * 512:1 + (tb + 1) * 512], kc == 0, kc == 7,
                                           r=[t_wbuf[wbi], t_h[kc]], w=[t_pp[pi]])
                                if m < 14:
                                    act(cx, Pst[pi][:, 1:S + 1], pp[pi][:], AF.Copy, r=[t_pp[pi]], w=[t_P[pi]])
                                    o, _ = VEC_COLS[f"mu{l}"]
                                    ts(cx, "dve", Tst_[:], Pst[pi][:, 0:S], vecs[:, o + m:o + m + 1], None, ALU.mult,
                                       r=[t_P[pi], t_vecs], w=[t_T])
                                    stt(cx, Ost[pi][:], Pst[pi][:, 1:S + 1], omm[:, l, m:m + 1], Tst_[:], ALU.mult, ALU.add,
                                        r=[t_P[pi], t_T, t_omm], w=[t_O[pi]])
                                elif 19 <= m < 35:
                                    act(cx, Ost[pi][:], pp[pi][:], AF.Sigmoid, r=[t_pp[pi]], w=[t_O[pi]])
                                else:
                                    act(cx, Ost[pi][:], pp[pi][:], AF.Copy, r=[t_pp[pi]], w=[t_O[pi]])
                                cx.dma("sp", projT[l][m * 128:(m + 1) * 128, :], Ost[pi][:], r=[t_O[pi]], w=[t_proj])
                        cx.barrier()
                xscope.close()
                xT = None
                cx.barrier()
                if stop == f"M1_{l}":
                    break
                with ExitStack() as ph:
                    waup = sb("waup", [128, 512], scope=ph); t_waup = Tok()
                    gup = sb("gup", [128, 512], scope=ph); t_gup = Tok()
                    cx.dma("sp", waup[:], waup_in[l, :, :], w=[t_waup])
                    cx.dma("sp", gup[:], gup_in[l, :, :], w=[t_gup])
                    TS = [sb(f"Tstate{i}", [64, 8, 64], scope=ph) for i in range(2)]; t_TS = toks(2)
                    TSb = [sb(f"TstateB{i}", [64, 8, 64], BF16, scope=ph) for i in range(2)]
                    cx.op("dve", lambda e: e.memset(TS[0][:], 0.0), w=[t_TS[0]])
                    cx.op("dve", lambda e: e.memset(TSb[0][:], 0.0), w=[t_TS[0]])
                    cur = 0
                    BT = 256
                    NCB = BT // 64
                    NP = 2
                    hv = lambda name: vecs[0:64, VEC_COLS[f"{name}{l}"][0]:VEC_COLS[f"{name}{l}"][0] + 8]
                    maskT = c64[:, 0, :, :]
                    maskAB = c64[:, 1, :, 0:64]
                    I8 = c64[:, 1, :, 64:128]
                    id64 = identb[0:64, 0:64]
                    o64 = ones64[:, 0:64]
                    t_yad = Tok()
                    for blk in range(S // BT):
                        c0 = blk * BT
                        with ExitStack() as pb:
                            names = ["R", "K", "V", "KK", "B", "E", "BTl", "KTl", "GA", "BO", "YT"]
                            T = {n: sb(n, [64, 8, BT], BF16 if n in ("BTl", "KTl") else F32, scope=pb) for n in names}
                            k = {n: Tok() for n in names}
                            for n in ("Rb", "Ab", "Bhb", "Khb", "Vb"):
                                T[n] = sb(n, [64, 8, BT], BF16, scope=pb)
                                k[n] = Tok()
                            WA = sb("WA", [128, BT], scope=pb); t_WA = Tok()
                            G_ = sb("G", [128, BT], scope=pb); t_G = Tok()
                            WC = sb("WC", [64, 8, NCB], scope=pb); t_WC = Tok()
                            Vtok = sb("Vtok", [64, NCB, 8, 64], BF16, scope=pb); t_Vtok = toks(NCB)
                            BHtok = sb("BHtok", [64, NCB, 8, 64], BF16, scope=pb); t_BHtok = toks(NCB)
                            KHtok = sb("KHtok", [64, NCB, 8, 64], BF16, scope=pb); t_KHtok = toks(NCB)
                            AbT = sb("AbT", [64, NCB, 8, 128], BF16, scope=pb); t_AbT = toks(NCB)
                            AkT = sb("AkT", [64, NCB, 8, 128], BF16, scope=pb); t_AkT = toks(NCB)
                            MT = sb("MT", [64, NCB, 8, 64], BF16, scope=pb); t_MT = toks(NCB)
                            for n, lo in (("R", 0), ("K", 512), ("V", 1024)):
                                cx.dma("sp", T[n][:], projT[l][lo:lo + 512, c0:c0 + BT].rearrange("(h n) t -> n h t", n=64), w=[k[n]])
                            cx.dma("sp", WA[:], projT[l][1536:1664, c0:c0 + BT], w=[t_WA])
                            cx.dma("sp", G_[:], projT[l][1664:1792, c0:c0 + BT], w=[t_G])
                            with ExitStack() as p2:
                                pA = ps("pA", [64, 8, BT], scope=p2); t_pA = Tok()
                                pB = ps("pB", [64, 8, BT], scope=p2); t_pB = Tok()
                                for n in ("IC", "LW", "CU"):
                                    T[n] = sb(n, [64, 8, BT], scope=p2)
                                    k[n] = Tok()
                                act(cx, WA[0:64, :], WA[0:64, :], AF.Tanh, r=[t_WA], w=[t_WA])
                                act(cx, G_[:], G_[:], AF.Sigmoid, r=[t_G], w=[t_G])
                                for h in range(8):
                                    mm(cx, pA[:, h, :], waup[0:64, h * 64:(h + 1) * 64], WA[0:64, :], True, True, r=[t_waup, t_WA], w=[t_pA])
                                for h in range(8):
                                    act(cx, T["LW"][:, h, :], pA[:, h, :], AF.Sigmoid, r=[t_pA, t_vecs], w=[k["LW"]], bias=hv("w0")[:, h:h + 1])
                                tap("t_LW", T["LW"][:], [k["LW"]])
                                for h in range(8):
                                    mm(cx, pB[:, h, :], waup[64:128, h * 64:(h + 1) * 64], WA[64:128, :], True, True, r=[t_waup, t_WA], w=[t_pB])
                                for h in range(8):
                                    act(cx, T["IC"][:, h, :], pB[:, h, :], AF.Sigmoid, r=[t_pB, t_vecs], w=[k["IC"]], bias=hv("a0")[:, h:h + 1])
                                for h in range(8):
                                    mm(cx, pA[:, h, :], gup[:, h * 64:(h + 1) * 64], G_[:], True, True, r=[t_gup, t_G], w=[t_pA])
                                cp(cx, "act", T["GA"][:], pA[:], r=[t_pA], w=[k["GA"]])
                                tap("t_IC", T["IC"][:], [k["IC"]])
                                tap("t_GA", T["GA"][:], [k["GA"]])
                                tt(cx, "dve", T["KK"][:], T["K"][:], bc3(hv("k_k"), BT), ALU.mult, r=[k["K"], t_vecs], w=[k["KK"]])
                                act(cx, T["E"][:], T["KK"][:], AF.Square, r=[k["KK"]], w=[k["E"]])
                                for h in range(8):
                                    mm(cx, pB[:, h, :], o64, T["E"][:, h, :], True, True, r=[k["E"], t_ones64], w=[t_pB])
                                act(cx, T["E"][:], pB[:], AF.Ln, r=[t_pB], w=[k["E"]], bias=1e-12)
                                act(cx, T["E"][:], T["E"][:], AF.Exp, r=[k["E"]], w=[k["E"]], scale=-0.5)
                                tt(cx, "dve", T["KK"][:], T["KK"][:], T["E"][:], ALU.mult, r=[k["KK"], k["E"]], w=[k["KK"]])
                                tt(cx, "dve", T["E"][:], T["IC"][:], bc3(hv("k_a"), BT), ALU.mult, r=[k["IC"], t_vecs], w=[k["E"]])
                                tt(cx, "dve", T["E"][:], T["E"][:], bc3(omka[:, l, :], BT), ALU.add, r=[k["E"], t_omka], w=[k["E"]])
                                tt(cx, "dve", T["K"][:], T["K"][:], T["E"][:], ALU.mult, r=[k["K"], k["E"]], w=[k["K"]])
                                tt(cx, "pool", T["B"][:], T["KK"][:], T["IC"][:], ALU.mult, r=[k["KK"], k["IC"]], w=[k["B"]])
                                tt(cx, "dve", T["E"][:], T["R"][:], bc3(hv("r_k"), BT), ALU.mult, r=[k["R"], t_vecs], w=[k["E"]])
                                tt(cx, "dve", T["E"][:], T["E"][:], T["K"][:], ALU.mult, r=[k["E"], k["K"]], w=[k["E"]])
                                for h in range(8):
                                    mm(cx, pA[:, h, :], o64, T["E"][:, h, :], True, True, r=[k["E"], t_ones64], w=[t_pA])
                                tt(cx, "dve", T["BO"][:], pA[:], T["V"][:], ALU.mult, r=[t_pA, k["V"]], w=[k["BO"]])
                                tap("t_KKn", T["KK"][:], [k["KK"]])
                                tap("t_Kp", T["K"][:], [k["K"]])
                                tap("t_BO", T["BO"][:], [k["BO"]])
                                for h in range(8):
                                    for c in range(NCB):
                                        cs = slice(c * 64, (c + 1) * 64)
                                        cx.op("dve", lambda e: e.tensor_tensor_scan(out=T["CU"][:, h, cs], data0=o64, data1=T["LW"][:, h, cs],
                                                                                    initial=0.0, op0=ALU.mult, op1=ALU.add),
                                              r=[k["LW"], t_ones64], w=[k["CU"]])
                                act(cx, T["E"][:], T["CU"][:], AF.Exp, r=[k["CU"]], w=[k["E"]], scale=-DECAY_SCALE)
                                tt(cx, "dve", T["Rb"][:], T["R"][:], T["E"][:], ALU.mult, r=[k["R"], k["E"]], w=[k["Rb"]])
                                cp(cx, "dve", WC[:], T["E"][:].rearrange("p h (c t) -> p h c t", t=64)[:, :, :, 63], r=[k["E"]], w=[t_WC])
                                tt(cx, "dve", T["E"][:], T["CU"][:], T["LW"][:], ALU.subtract, r=[k["CU"], k["LW"]], w=[k["E"]])
                                act(cx, T["E"][:], T["E"][:], AF.Exp, r=[k["E"]], w=[k["E"]], scale=-DECAY_SCALE)
                                stt(cx, T["Ab"][:], T["KK"][:], -1.0, T["E"][:], ALU.mult, ALU.mult, r=[k["KK"], k["E"]], w=[k["Ab"]])
                                act(cx, T["E"][:], T["CU"][:], AF.Exp, r=[k["CU"]], w=[k["E"]], scale=DECAY_SCALE)
                                tt(cx, "dve", T["BTl"][:], T["B"][:], T["E"][:], ALU.mult, r=[k["B"], k["E"]], w=[k["BTl"]])
                                tt(cx, "dve", T["KTl"][:], T["K"][:], T["E"][:], ALU.mult, r=[k["K"], k["E"]], w=[k["KTl"]])
                                CUv = T["CU"][:].rearrange("p h (c t) -> p (h c) t", t=64)
                                Ev = T["E"][:].rearrange("p h (c t) -> p (h c) t", t=64)
                                tt(cx, "dve", Ev, CUv[:, :, 63:64].to_broadcast([64, 8 * NCB, 64]), CUv, ALU.subtract, r=[k["CU"]], w=[k["E"]])
                                act(cx, T["E"][:], T["E"][:], AF.Exp, r=[k["E"]], w=[k["E"]], scale=-DECAY_SCALE)
                                tt(cx, "dve", T["Bhb"][:], T["B"][:], T["E"][:], ALU.mult, r=[k["B"], k["E"]], w=[k["Bhb"]])
                                tt(cx, "pool", T["Khb"][:], T["K"][:], T["E"][:], ALU.mult, r=[k["K"], k["E"]], w=[k["Khb"]])
                                cp(cx, "act", T["Vb"][:], T["V"][:], r=[k["V"]], w=[k["Vb"]])
                                tap("t_CU", T["CU"][:], [k["CU"]])
                                tap("t_Rt", T["R"][:], [k["R"]])
                                tap("t_At", T["KK"][:], [k["KK"]])
                                tap("t_Bt", T["BTl"][:], [k["BTl"]])
                                tap("t_Kt", T["KTl"][:], [k["KTl"]])
                                tap("t_Bh", T["B"][:], [k["B"]])
                                tap("t_Kh", T["K"][:], [k["K"]])
                                tap("t_WC", WC[:], [t_WC])
                                cx.barrier()
                            with ExitStack() as p2:
                                pT_ = ps("pT", [64, 8, 64], BF16, scope=p2); t_pT = Tok()
                                pG = [ps(f"pG{i}", [64, NP, 8, 64], scope=p2) for i in range(3)]; t_pG = toks(3)
                                pAb = pG[1][:].rearrange("p c h t -> p (c h t)").rearrange("p (h x) -> p h x", x=128)
                                pAk = pG[2][:].rearrange("p c h t -> p (c h t)").rearrange("p (h x) -> p h x", x=128)
                                Pm = [sb(f"Pm{i}", [64, NP, 8, 64], BF16, scope=p2) for i in range(2)]; t_Pm = toks(2)
                                PTm = [sb(f"PTm{i}", [64, NP, 8, 64], BF16, scope=p2) for i in range(2)]; t_PTm = toks(2)
                                IPm = sb("IPm", [64, NP, 8, 64], BF16, scope=p2); t_IPm = Tok()
                                Rm = [sb(f"Rm{i}", [64, NP, 8, 64], BF16, scope=p2) for i in range(2)]; t_Rm = toks(2)
                                Xsb = sb("Xsb", [64, 8, 64], BF16, scope=p2); t_Xsb = Tok()
                                Usb = sb("Usb", [64, 8, 64], BF16, scope=p2); t_Usb = Tok()
                                Ttmp = sb("Ttmp", [64, 8, 64], scope=p2); t_Ttmp = Tok()
                                maskAB4 = maskAB.unsqueeze(1).to_broadcast([64, NP, 8, 64])
                                I84 = I8.unsqueeze(1).to_broadcast([64, NP, 8, 64])
                                for pr in range(NCB // NP):
                                    for c2 in range(NP):
                                        c = pr * NP + c2
                                        cs = slice(c * 64, (c + 1) * 64)
                                        for (src, dst, tdst) in (("Vb", Vtok, t_Vtok), ("Bhb", BHtok, t_BHtok), ("Khb", KHtok, t_KHtok)):
                                            for h in range(8):
                                                cx.op("pe", lambda e: e.transpose(out=pT_[:, h, :], in_=T[src][:, h, cs], identity=id64),
                                                      r=[k[src], t_consts], w=[t_pT])
                                            cp(cx, "act", dst[:, c, :, :], pT_[:], r=[t_pT], w=[tdst[c]])
                                        for h in range(8):
                                            mm(cx, pAb[:, h, 0:64], T["BTl"][:, h, cs], T["Ab"][:, h, cs], True, True, r=[k["BTl"], k["Ab"]], w=[t_pG[1]])
                                            mm(cx, pAb[:, h, 64:128], T["BTl"][:, h, cs], T["Rb"][:, h, cs], True, True, r=[k["BTl"], k["Rb"]], w=[t_pG[1]])
                                            mm(cx, pAk[:, h, 0:64], T["KTl"][:, h, cs], T["Ab"][:, h, cs], True, True, r=[k["KTl"], k["Ab"]], w=[t_pG[2]])
                                            mm(cx, pAk[:, h, 64:128], T["KTl"][:, h, cs], T["Rb"][:, h, cs], True, True, r=[k["KTl"], k["Rb"]], w=[t_pG[2]])
                                            mm(cx, pG[0][:, c2, h, :], T["Ab"][:, h, cs], T["BTl"][:, h, cs], True, True, r=[k["BTl"], k["Ab"]], w=[t_pG[0]])
                                        tt(cx, "dve", AbT[:, c, :, :], pAb, maskT, ALU.mult, r=[t_pG[1], t_c64], w=[t_AbT[c]])
                                        tt(cx, "dve", AkT[:, c, :, :], pAk, maskT, ALU.mult, r=[t_pG[2], t_c64], w=[t_AkT[c]])
                                    tt(cx, "dve", Pm[0][:], pG[0][:], maskAB4, ALU.mult, r=[t_pG[0], t_c64], w=[t_Pm[0]])
                                    cp(cx, "act", PTm[0][:], AbT[:, pr * NP:(pr + 1) * NP, :, 0:64], r=t_AbT[pr * NP:(pr + 1) * NP], w=[t_PTm[0]])
                                    tt(cx, "dve", Rm[0][:], AbT[:, pr * NP:(pr + 1) * NP, :, 0:64], I84, ALU.add, r=t_AbT[pr * NP:(pr + 1) * NP] + [t_c64], w=[t_Rm[0]])

                                    def mmP(pi_):
                                        for c in range(NP):
                                            for h in range(8):
                                                mm(cx, pG[0][:, c, h, :], PTm[pi_][:, c, h, :], Pm[pi_][:, c, h, :], True, True, r=[t_PTm[pi_], t_Pm[pi_]], w=[t_pG[0]])

                                    def mmPT(pi_):
                                        for c in range(NP):
                                            for h in range(8):
                                                mm(cx, pG[1][:, c, h, :], Pm[pi_][:, c, h, :], PTm[pi_][:, c, h, :], True, True, r=[t_PTm[pi_], t_Pm[pi_]], w=[t_pG[1]])

                                    def mmR(ri_):
                                        for c in range(NP):
                                            for h in range(8):
                                                mm(cx, pG[2][:, c, h, :], IPm[:, c, h, :], Rm[ri_][:, c, h, :], True, True, r=[t_IPm, t_Rm[ri_]], w=[t_pG[2]])

                                    pi = 0
                                    ri = 0
                                    for kk_ in range(1, 6):
                                        ni = 1 - pi
                                        mmP(pi)
                                        if kk_ < 5:
                                            mmPT(pi)
                                        if kk_ > 1:
                                            mmR(ri)
                                        if kk_ < 5:
                                            cp(cx, "act", PTm[ni][:], pG[1][:], r=[t_pG[1]], w=[t_PTm[ni]])
                                        cp(cx, "act", Pm[ni][:], pG[0][:], r=[t_pG[0]], w=[t_Pm[ni]])
                                        if kk_ > 1:
                                            cp(cx, "dve", Rm[1 - ri][:], pG[2][:], r=[t_pG[2]], w=[t_Rm[1 - ri]])
                                            ri = 1 - ri
                                        tt(cx, "dve", IPm[:], Pm[ni][:], I84, ALU.add, r=[t_Pm[ni], t_c64], w=[t_IPm])
                                        pi = ni
                                    mmR(ri)
                                    cp(cx, "dve", MT[:, pr * NP:(pr + 1) * NP, :, :], pG[2][:], r=[t_pG[2]], w=t_MT[pr * NP:(pr + 1) * NP])
                                for c in range(NCB):
                                    cs = slice(c * 64, (c + 1) * 64)
                                    To, Tn = TS[cur], TS[1 - cur]
                                    ToB, TnB = TSb[cur], TSb[1 - cur]
                                    t_To, t_Tn = t_TS[cur], t_TS[1 - cur]
                                    pX = pG[0][:, 0, :, :]
                                    pU = pG[0][:, 1, :, :]
                                    pTn = pG[1][:, 0, :, :]
                                    pY = pG[2][:, 0, :, :]
                                    for h in range(8):
                                        mm(cx, pX[:, h, :], T["Ab"][:, h, cs], ToB[:, h, :], True, False, r=[k["Ab"], t_To], w=[t_pG[0]])
                                        mm(cx, pX[:, h, :], AkT[:, c, h, 0:64], Vtok[:, c, h, :], False, True, r=[t_AkT[c], t_Vtok[c]], w=[t_pG[0]])
                                    cp(cx, "act", Xsb[:], pX, r=[t_pG[0]], w=[t_Xsb])
                                    for h in range(8):
                                        mm(cx, pU[:, h, :], MT[:, c, h, :], Xsb[:, h, :], True, True, r=[t_MT[c], t_Xsb], w=[t_pG[0]])
                                    cp(cx, "act", Usb[:], pU, r=[t_pG[0]], w=[t_Usb])
                                    for h in range(8):
                                        mm(cx, pTn[:, h, :], BHtok[:, c, h, :], Usb[:, h, :], True, False, r=[t_BHtok[c], t_Usb], w=[t_pG[1]])
                                        mm(cx, pTn[:, h, :], KHtok[:, c, h, :], Vtok[:, c, h, :], False, True, r=[t_KHtok[c], t_Vtok[c]], w=[t_pG[1]])
                                    tt(cx, "dve", Ttmp[:], To[:], bc3(WC[:, :, c], 64), ALU.mult, r=[t_To, t_WC], w=[t_Ttmp])
                                    tt(cx, "dve", TnB[:], Ttmp[:], pTn, ALU.add, r=[t_Ttmp, t_pG[1]], w=[t_Tn])
                                    tt(cx, "dve", Tn[:], Ttmp[:], pTn, ALU.add, r=[t_Ttmp, t_pG[1], t_Tn], w=[t_Tn])
                                    for h in range(8):
                                        mm(cx, pY[:, h, :], ToB[:, h, :], T["Rb"][:, h, cs], True, False, r=[k["Rb"], t_To], w=[t_pG[2]])
                                        mm(cx, pY[:, h, :], Usb[:, h, :], AbT[:, c, h, 64:128], False, False, r=[t_AbT[c], t_Usb], w=[t_pG[2]])
                                        mm(cx, pY[:, h, :], Vtok[:, c, h, :], AkT[:, c, h, 64:128], False, True, r=[t_AkT[c], t_Vtok[c]], w=[t_pG[2]])
                                    cp(cx, "act", T["YT"][:, :, cs], pY, r=[t_pG[2]], w=[k["YT"]])
                                    cur = 1 - cur
                                cx.barrier()
                            with ExitStack() as p2:
                                pA = ps("pA2", [64, 8, BT], scope=p2); t_pA = Tok()
                                for h in range(8):
                                    mm(cx, pA[:, h, :], o64, T["YT"][:, h, :], True, True, r=[k["YT"], t_ones64], w=[t_pA])
                                stt(cx, T["YT"][:], pA[:], -1.0 / 64, T["YT"][:], ALU.mult, ALU.add, r=[t_pA, k["YT"]], w=[k["YT"]])
                                act(cx, T["E"][:], T["YT"][:], AF.Square, r=[k["YT"]], w=[k["E"]])
                                for h in range(8):
                                    mm(cx, pA[:, h, :], o64, T["E"][:, h, :], True, True, r=[k["E"], t_ones64], w=[t_pA])
                                act(cx, T["E"][:], pA[:], AF.Ln, r=[t_pA], w=[k["E"]], bias=LNX_EPS, scale=1.0 / 64)
                                act(cx, T["E"][:], T["E"][:], AF.Exp, r=[k["E"]], w=[k["E"]], scale=-0.5)
                                tt(cx, "dve", T["YT"][:], T["YT"][:], T["E"][:], ALU.mult, r=[k["YT"], k["E"]], w=[k["YT"]])
                                tt(cx, "dve", T["YT"][:], T["YT"][:], bc3(hv("lnx_g"), BT), ALU.mult, r=[k["YT"], t_vecs], w=[k["YT"]])
                                tt(cx, "dve", T["YT"][:], T["YT"][:], bc3(hv("lnx_b"), BT), ALU.add, r=[k["YT"], t_vecs], w=[k["YT"]])
                                tt(cx, "dve", T["YT"][:], T["YT"][:], T["BO"][:], ALU.add, r=[k["YT"], k["BO"]], w=[k["YT"]])
                                tt(cx, "dve", T["Rb"][:], T["YT"][:], T["GA"][:], ALU.mult, r=[k["YT"], k["GA"], k["Rb"]], w=[k["Rb"]])
                                cx.dma("sp", yaT_d[l][:, c0:c0 + BT].rearrange("(h n) t -> n h t", n=64), T["Rb"][:], r=[k["Rb"]], w=[t_yad])
                                cx.barrier()
                    cx.barrier()
                if stop == f"M2_{l}":
                    break
                with ExitStack() as ph:
                    rr = slice(64, 96)
                    CC = sb("CC", [96, S], scope=ph); t_CC = Tok()
                    SS = sb("SS", [96, S], scope=ph); t_SS = Tok()
                    ropef = vecs[:, VEC_COLS["ropef"][0]:VEC_COLS["ropef"][0] + 1]
                    ropes = vecs[:, VEC_COLS["ropes"][0]:VEC_COLS["ropes"][0] + 1]
                    if l == 0:
                        with ExitStack() as p2:
                            posi = sb("posi", [96, S], I32, scope=p2); t_posi = Tok()
                            ang = sb("ang", [96, S], scope=p2); t_ang = Tok()
                            tq = sb("tq", [96, S], scope=p2); t_tq = Tok()
                            cx.dma("sp", posi[rr, :], pos_in[:, :], w=[t_posi])
                            cp(cx, "dve", ang[rr, :], posi[rr, :], r=[t_posi], w=[t_ang])
                            ts(cx, "dve", ang[rr, :], ang[rr, :], ropef[rr, :], None, ALU.mult, r=[t_ang, t_vecs], w=[t_ang])
                            MAGIC = 12582912.0
                            C1 = 6.28125
                            C2 = 2.0 * math.pi - 6.28125
                            for (dst, tdst, phase) in ((SS, t_SS, 0.0), (CC, t_CC, math.pi / 2)):
                                ts(cx, "dve", tq[rr, :], ang[rr, :], phase, 1.0 / (2.0 * math.pi), ALU.add, ALU.mult, r=[t_ang], w=[t_tq])
                                ts(cx, "dve", tq[rr, :], tq[rr, :], MAGIC, None, ALU.add, r=[t_tq], w=[t_tq])
                                ts(cx, "dve", tq[rr, :], tq[rr, :], -MAGIC, None, ALU.add, r=[t_tq], w=[t_tq])
                                stt(cx, dst[rr, :], tq[rr, :], -C1, ang[rr, :], ALU.mult, ALU.add, r=[t_tq, t_ang], w=[tdst])
                                stt(cx, dst[rr, :], tq[rr, :], -C2, dst[rr, :], ALU.mult, ALU.add, r=[t_tq, tdst], w=[tdst])
                                ts(cx, "dve", dst[rr, :], dst[rr, :], phase, 3.1415925, ALU.add, ALU.min, r=[tdst], w=[tdst])
                                ts(cx, "dve", dst[rr, :], dst[rr, :], -3.1415925, None, ALU.max, r=[tdst], w=[tdst])
                                act(cx, dst[rr, :], dst[rr, :], AF.Sin, r=[tdst], w=[tdst])
                            ts(cx, "dve", SS[rr, :], SS[rr, :], ropes[rr, :], None, ALU.mult, r=[t_SS, t_vecs], w=[t_SS])
                            cx.dma("sp", rope_d[0, :, :], CC[rr, :], r=[t_CC], w=[t_rope])
                            cx.dma("sp", rope_d[1, :, :], SS[rr, :], r=[t_SS], w=[t_rope])
                            cx.barrier()
                    else:
                        cx.dma("sp", CC[rr, :], rope_d[0, :, :], r=[t_rope], w=[t_CC])
                        cx.dma("sp", SS[rr, :], rope_d[1, :, :], r=[t_rope], w=[t_SS])
                    QN = sb("QN", [128, 3, S], BF16, scope=ph); t_QN = Tok()
                    KVN = sb("KVN", [128, 2, S], BF16, scope=ph); t_KVN = Tok()
                    KPE = sb("KPE", [96, S], BF16, scope=ph); t_KPE = Tok()
                    VT = sb("VT", [128, 16, 512], BF16, scope=ph); t_VT = Tok()
                    wqb = sb("wqb", [128, 3, 768], BF16, scope=ph); t_wqb = Tok()
                    wqbs = sb("wqbs", [128, 3, 768], BF16, scope=ph); t_wqbs = Tok()
                    wkvk = sb("wkvk", [128, 2, 512], BF16, scope=ph); t_wkvk = Tok()
                    wkvv = sb("wkvv", [128, 2, 512], BF16, scope=ph); t_wkvv = Tok()
                    cx.dma("pool", wqb[:], wqb_in[l, :, :, :], w=[t_wqb])
                    cx.dma("pool", wqbs[:], wqbs_in[l, :, :, :], w=[t_wqbs])
                    cx.dma("pool", wkvk[:], wkvk_in[l, :, :, :], w=[t_wkvk])
                    cx.dma("pool", wkvv[:], wkvv_in[l, :, :, :], w=[t_wkvv])
                    with ExitStack() as p2:
                        raw = sb("raw", [128, 3, 512], scope=p2); t_raw = Tok()
                        sq = sb("sqm", [128, 512], scope=p2); t_sq = Tok()
                        rs_ = sb("rsm", [128, 512], scope=p2); t_rs = Tok()
                        tmpm = sb("tmpm", [128, 512], scope=p2); t_tmpm = Tok()
                        pss = ps("pssm", [128, 512], scope=p2); t_pss = Tok()
                        for (dst, tdst, nchk, row0, gname) in ((QN, t_QN, 3, 1792, "q_norm"), (KVN, t_KVN, 2, 2176, "kv_norm")):
                            go = VEC_COLS[f"{gname}{l}"][0]
                            for tb in range(4):
                                cs = slice(tb * 512, (tb + 1) * 512)
                                cx.dma("sp", raw[:, 0:nchk, :], projT[l][row0:row0 + nchk * 128, cs].rearrange("(c p) t -> p c t", p=128), w=[t_raw])
                                for c in range(nchk):
                                    act(cx, sq[:], raw[:, c, :], AF.Square, r=[t_raw], w=[t_sq])
                                    mm(cx, pss[:], ones128, sq[:], c == 0, c == nchk - 1, r=[t_sq, t_consts], w=[t_pss])
                                act(cx, rs_[:], pss[:], AF.Sqrt, r=[t_pss], w=[t_rs], bias=NORM_EPS, scale=1.0 / (nchk * 128))
                                cx.op("dve", lambda e: e.reciprocal(out=rs_[:], in_=rs_[:]), r=[t_rs], w=[t_rs])
                                for c in range(nchk):
                                    tt(cx, "dve", tmpm[:], raw[:, c, :], rs_[:], ALU.mult, r=[t_raw, t_rs], w=[t_tmpm])
                                    act(cx, dst[:, c, cs], tmpm[:], AF.Copy, r=[t_tmpm, t_vecs], w=[tdst], scale=vecs[:, go + c:go + c + 1])
                        KR = sb("KR", [96, S], scope=p2); t_KR = Tok()
                        KRs = sb("KRs", [96, S], scope=p2); t_KRs = Tok()
                        kb = 35 * 128
                        cx.dma("sp", KR[64:96, :], projT[l][kb + 64:kb + 96, :], w=[t_KR])
                        cx.dma("sp", KRs[64:80, :], projT[l][kb + 80:kb + 96, :], w=[t_KRs])
                        cx.dma("sp", KRs[80:96, :], projT[l][kb + 64:kb + 80, :], w=[t_KRs])
                        tt(cx, "dve", KR[rr, :], KR[rr, :], CC[rr, :], ALU.mult, r=[t_KR, t_CC], w=[t_KR])
                        tt(cx, "dve", KRs[rr, :], KRs[rr, :], SS[rr, :], ALU.mult, r=[t_KRs, t_SS], w=[t_KRs])
                        tt(cx, "dve", KPE[rr, :], KR[rr, :], KRs[rr, :], ALU.add, r=[t_KR, t_KRs], w=[t_KPE])
                        pV = ps("pV", [128, 512], scope=p2); t_pV = Tok()
                        for tt_ in range(16):
                            for kc in range(2):
                                mm(cx, pV[:], KVN[:, kc, tt_ * 128:(tt_ + 1) * 128], wkvv[:, kc, :], kc == 0, kc == 1, r=[t_KVN, t_wkvv], w=[t_pV])
                            cp(cx, "act", VT[:, tt_, :], pV[:], r=[t_pV], w=[t_VT])
                        cx.barrier()
                    with ExitStack() as p2:
                        QH = [sb(f"QH{i}", [96, S], BF16, scope=p2) for i in range(2)]; t_QH = toks(2)
                        KH = [sb(f"KH{i}", [96, S], BF16, scope=p2) for i in range(2)]; t_KH = toks(2)
                        PT = [sb(f"PT{i}", [128, 16, 128], BF16, scope=p2) for i in range(2)]; t_PT = toks(2)
                        T1 = sb("T1", [96, 512], scope=p2); t_T1 = Tok()
                        T2 = sb("T2", [96, 512], scope=p2); t_T2 = Tok()
                        OTh = [sb(f"OTh{i}", [64, S], BF16, scope=p2) for i in range(2)]; t_OTh = toks(2)
                        RD = sb("RD", [64, 128], scope=p2); t_RD = Tok()
                        mlam = sb("mlam", [128, 128], BF16, scope=p2); t_mlam = Tok()
                        cp(cx, "dve", mlam[:], mlamask, r=[t_consts], w=[t_mlam])
                        psQ = ps("psQ", [96, 512], scope=p2); t_psQ = Tok()
                        psQs = ps("psQs", [96, 512], scope=p2); t_psQs = Tok()
                        psK = ps("psK", [64, 512], scope=p2); t_psK = Tok()
                        psS = [ps(f"psS{i}", [128, 4, 128], scope=p2) for i in range(2)]; t_psS = toks(2)
                        psOD = [ps(f"psOD{i}", [64, 2, 128], scope=p2) for i in range(2)]; t_psOD = toks(2)
                        t_od = Tok()
                        si_box = [0]
                        def proj_tb(h, tb):
                            hb = h % 2
                            cs = slice(tb * 512, (tb + 1) * 512)
                            for kc in range(3):
                                mm(cx, psQ[:], wqb[:, kc, h * 96:(h + 1) * 96], QN[:, kc, cs], kc == 0, kc == 2, r=[t_wqb, t_QN], w=[t_psQ])
                            for kc in range(3):
                                mm(cx, psQs[:], wqbs[:, kc, h * 96:(h + 1) * 96], QN[:, kc, cs], kc == 0, kc == 2, r=[t_wqbs, t_QN], w=[t_psQs])
                            for kc in range(2):
                                mm(cx, psK[:], wkvk[:, kc, h * 64:(h + 1) * 64], KVN[:, kc, cs], kc == 0, kc == 1, r=[t_wkvk, t_KVN], w=[t_psK])
                            tt(cx, "dve", T1[rr, :], psQ[rr, :], CC[rr, cs], ALU.mult, r=[t_psQ, t_CC], w=[t_T1])
                            tt(cx, "dve", T2[rr, :], psQs[rr, :], SS[rr, cs], ALU.mult, r=[t_psQs, t_SS], w=[t_T2])
                            cp(cx, "act", QH[hb][0:64, cs], psQ[0:64, :], r=[t_psQ, t_T1], w=[t_QH[hb]])
                            tt(cx, "pool", QH[hb][rr, cs], T1[rr, :], T2[rr, :], ALU.add, r=[t_T1, t_T2], w=[t_QH[hb]])
                            cp(cx, "act", KH[hb][0:64, cs], psK[:], r=[t_psK], w=[t_KH[hb]])
                        def proj_fin(h):
                            hb = h % 2
                            cp(cx, "pool", KH[hb][rr, :], KPE[rr, :], r=[t_KPE], w=[t_KH[hb]])
                        for tb in range(4):
                            proj_tb(0, tb)
                        proj_fin(0)
                        for h in range(8):
                            hb = h % 2
                            def s_part(qb, h=h, hb=hb):
                                nk = qb + 1
                                qs = slice(qb * 128, (qb + 1) * 128)
                                pt = PT[qb % 2]; t_pt = t_PT[qb % 2]
                                for g in range((nk + 3) // 4):
                                    pS = psS[si_box[0]]; t_pS = t_psS[si_box[0]]
                                    si_box[0] = 1 - si_box[0]
                                    n_in = min(4, nk - g * 4)
                                    for j in range(n_in):
                                        kt = g * 4 + j
                                        mm(cx, pS[:, j, :], KH[hb][:, kt * 128:(kt + 1) * 128], QH[hb][:, qs], True, True, r=[t_KH[hb], t_QH[hb]], w=[t_pS])
                                    act(cx, pt[:, g * 4:g * 4 + n_in, :], pS[:, 0:n_in, :], AF.Exp, r=[t_pS], w=[t_pt], scale=96.0 ** -0.5)
                                tt(cx, "pool", pt[:, qb, :], pt[:, qb, :], mlam[:], ALU.mult, r=[t_pt, t_mlam], w=[t_pt])
                            def od_part(qb, h=h, hb=hb):
                                nk = qb + 1
                                qs = slice(qb * 128, (qb + 1) * 128)
                                pt = PT[qb % 2]; t_pt = t_PT[qb % 2]
                                pOD = psOD[qb % 2]; t_pOD = t_psOD[qb % 2]
                                for kt in range(nk):
                                    mm(cx, pOD[:, 0, :], VT[:, kt, h * 64:(h + 1) * 64], pt[:, kt, :], kt == 0, kt == nk - 1, r=[t_VT, t_pt], w=[t_pOD])
                                for kt in range(nk):
                                    mm(cx, pOD[:, 1, :], onesb[:, 0:64], pt[:, kt, :], kt == 0, kt == nk - 1, r=[t_onesb, t_pt], w=[t_pOD])
                                cx.op("dve", lambda e: e.reciprocal(out=RD[:], in_=pOD[:, 1, :]), r=[t_pOD], w=[t_RD])
                                tt(cx, "dve", OTh[hb][:, qs], pOD[:, 0, :], RD[:], ALU.mult, r=[t_pOD, t_RD], w=[t_OTh[hb]])
                            s_part(0)
                            for qb in range(16):
                                if qb + 1 < 16:
                                    s_part(qb + 1)
                                od_part(qb)
                                if h + 1 < 8 and qb in (2, 5, 8, 11):
                                    proj_tb(h + 1, (2, 5, 8, 11).index(qb))
                                if h + 1 < 8 and qb == 12:
                                    proj_fin(h + 1)
                            cx.dma("sp", oT_d[l][h * 64:(h + 1) * 64, :], OTh[hb][:], r=[t_OTh[hb]], w=[t_od])
                        cx.barrier()
                if stop == f"M3_{l}":
                    break
                xscope = ExitStack()
                xT = sb("xT", [128, 8, S], scope=xscope)
                for kc in range(8):
                    cx.dma("sp", xT[:, kc, :], xs_d[kc * 128:(kc + 1) * 128, :], r=[t_xs], w=[t_x[kc]])
                with ExitStack() as ph:
                    ROUT = sb("ROUT", [64, 8, D], BF16, scope=ph); t_ROUT = Tok()
                    MOUT = sb("MOUT", [64, 8, D], BF16, scope=ph); t_MOUT = Tok()
                    WO = sb("WO", [128, 8, D], BF16, scope=ph); t_WO = Tok()
                    YAs = [sb(f"YA{i}", [64, 8, 512], BF16, scope=ph) for i in range(2)]; t_YAs = toks(2)
                    OTbs = [sb(f"OTb{i}", [64, 8, 512], BF16, scope=ph) for i in range(2)]; t_OTbs = toks(2)
                    stg4 = Stager(ph, 2048, 2)
                    for pc in range(4):
                        stg4.load(ROUT[:, pc * 2:(pc + 1) * 2, :].rearrange("p a m -> p (a m)"),
                                  rout_in[l, :, pc * 2:(pc + 1) * 2, :].rearrange("p a m -> p (a m)"), "dve", [t_ROUT])
                        stg4.load(MOUT[:, pc * 2:(pc + 1) * 2, :].rearrange("p a m -> p (a m)"),
                                  mout_in[l, :, pc * 2:(pc + 1) * 2, :].rearrange("p a m -> p (a m)"), "act", [t_MOUT])
                    for pc in range(4):
                        stg4.load(WO[:, pc * 2:(pc + 1) * 2, :].rearrange("p a m -> p (a m)"),
                                  wo_in[l, :, pc * 2:(pc + 1) * 2, :].rearrange("p a m -> p (a m)"), "dve" if pc % 2 == 0 else "act", [t_WO])
                    GAg = [sb(f"GAg{i}", [128, 512], scope=ph) for i in range(2)]; t_GAg = toks(2)
                    GBg = [sb(f"GBg{i}", [128, 512], scope=ph) for i in range(2)]; t_GBg = toks(2)
                    T1m = [sb(f"T1m{i}", [128, 512], scope=ph) for i in range(2)]; t_T1m = toks(2)
                    T2m = [sb(f"T2m{i}", [128, 512], scope=ph) for i in range(2)]; t_T2m = toks(2)
                    MG = sb("MG", [128, 8, 512], BF16, scope=ph); t_MG = toks(8)
                    psA = [ps(f"psA{i}", [128, 512], scope=ph) for i in range(2)]; t_psA = toks(2)
                    psB = [ps(f"psB{i}", [128, 512], scope=ph) for i in range(2)]; t_psB = toks(2)
                    psC = [ps(f"psC{i}", [128, 512], scope=ph) for i in range(2)]; t_psC = toks(2)
                    gcol = mods[:, l * 2, 16:24]
                    for tb in range(4):
                        cs = slice(tb * 512, (tb + 1) * 512)
                        YA = YAs[tb % 2]; t_YA = t_YAs[tb % 2]
                        OTb = OTbs[tb % 2]; t_OTb = t_OTbs[tb % 2]
                        cx.dma("sp", YA[:], yaT_d[l][:, cs].rearrange("(h n) t -> n h t", n=64), r=[t_yad], w=[t_YA])
                        cx.dma("sp", OTb[:], oT_d[l][:, cs].rearrange("(h n) t -> n h t", n=64), r=[t_od], w=[t_OTb])
                        for mc in range(8):
                            i = mc % 2
                            cx.dma("sp", GAg[i][:], projT[l][(19 + mc) * 128:(20 + mc) * 128, cs], w=[t_GAg[i]])
                            cx.dma("sp", GBg[i][:], projT[l][(27 + mc) * 128:(28 + mc) * 128, cs], w=[t_GBg[i]])
                            for h in range(8):
                                mm(cx, psA[i][:], ROUT[:, h, mc * 128:(mc + 1) * 128], YA[:, h, :], h == 0, h == 7, r=[t_ROUT, t_YA], w=[t_psA[i]])
                            for h in range(8):
                                mm(cx, psB[i][:], MOUT[:, h, mc * 128:(mc + 1) * 128], OTb[:, h, :], h == 0, h == 7, r=[t_MOUT, t_OTb], w=[t_psB[i]])
                            tt(cx, "dve", T1m[i][:], psA[i][:], GAg[i][:], ALU.mult, r=[t_psA[i], t_GAg[i]], w=[t_T1m[i]])
                            tt(cx, "dve", T2m[i][:], psB[i][:], GBg[i][:], ALU.mult, r=[t_psB[i], t_GBg[i]], w=[t_T2m[i]])
                            tt(cx, "pool", MG[:, mc, :], T1m[i][:], T2m[i][:], ALU.add, r=[t_T1m[i], t_T2m[i]], w=[t_MG[mc]])
                        for oc in range(8):
                            j = oc % 2
                            for mc in range(8):
                                mm(cx, psC[j][:], WO[:, mc, oc * 128:(oc + 1) * 128], MG[:, mc, :], mc == 0, mc == 7, r=[t_WO, t_MG[mc]], w=[t_psC[j]])
                            stt(cx, xT[:, oc, cs], psC[j][:], gcol[:, oc:oc + 1], xT[:, oc, cs], ALU.mult, ALU.add,
                                r=[t_psC[j], t_x[oc], t_mods], w=[t_x[oc]])
                    cx.barrier()
                if stop == f"M4_{l}":
                    for kc in range(8):
                        cx.dma("sp", xs_d[kc * 128:(kc + 1) * 128, :], xT[:, kc, :], r=[t_x[kc]], w=[t_xs])
                    cx.barrier()
                    break
                moe = (l % 2 == 1)
                with ExitStack() as ph:
                    hT = sb("hT2", [128, 8, S + 1], BF16, scope=ph); t_h = toks(8)
                    CT = sb("CT", [8, S], scope=ph); t_CT = Tok()
                    with ExitStack() as p2:
                        if moe:
                            h32 = sb("h32", [128, 8, 512], scope=p2); t_h32 = Tok()
                            LOG = sb("LOG", [128, 16, 8], scope=p2); t_LOG = Tok()
                            RW = sb("RW", [128, 8, 8], scope=p2); t_RW = Tok()
                            cx.dma("sp", RW[:], rw_in[:, :, :], w=[t_RW])
                            psL = ps("psL", [128, 512], scope=p2); t_psL = Tok()
                            rbo = VEC_COLS["router_b"][0]

                            def hook(tb):
                                for tsub in range(4):
                                    ti = tb * 4 + tsub
                                    for kc in range(8):
                                        mm(cx, psL[:, 0:8], h32[:, kc, tsub * 128:(tsub + 1) * 128], RW[:, kc, :], kc == 0, kc == 7,
                                           r=[t_h32, t_RW], w=[t_psL])
                                    tt(cx, "dve", LOG[:, ti, :], psL[:, 0:8], vecs[:, rbo:rbo + 8], ALU.add, r=[t_psL, t_vecs], w=[t_LOG])
                            rms_to_h(p2, xT, t_x, hT, t_h, acol[:, l * 2 + 1, :], mods[:, l * 2 + 1, 0:8], h32=h32, t_h32=t_h32, tb_hook=hook)
                            m1 = sb("m1", [128, 16], scope=p2); m2 = sb("m2", [128, 16], scope=p2)
                            eq1 = sb("eq1", [128, 16, 8], scope=p2); eq2 = sb("eq2", [128, 16, 8], scope=p2)
                            L2 = sb("L2", [128, 16, 8], scope=p2); COMB = sb("COMB", [128, 16, 8], scope=p2)
                            dd = sb("dd", [128, 16], scope=p2); p1 = sb("p1", [128, 16], scope=p2); p2_ = sb("p2_", [128, 16], scope=p2)
                            tk = Tok()
                            R_ = [t_LOG, tk]
                            cx.op("dve", lambda e: e.tensor_reduce(out=m1[:], in_=LOG[:], axis=AX.X, op=ALU.max), r=R_, w=[tk])
                            tt(cx, "dve", eq1[:], LOG[:], bc3(m1[:], 8), ALU.is_equal, r=R_, w=[tk])
                            stt(cx, L2[:], eq1[:], -1e30, LOG[:], ALU.mult, ALU.add, r=R_, w=[tk])
                            cx.op("dve", lambda e: e.tensor_reduce(out=m2[:], in_=L2[:], axis=AX.X, op=ALU.max), r=R_, w=[tk])
                            tt(cx, "dve", eq2[:], L2[:], bc3(m2[:], 8), ALU.is_equal, r=R_, w=[tk])
                            tt(cx, "dve", dd[:], m2[:], m1[:], ALU.subtract, r=R_, w=[tk])
                            act(cx, dd[:], dd[:], AF.Exp, r=R_, w=[tk])
                            ts(cx, "dve", p1[:], dd[:], 1.0, None, ALU.add, r=R_, w=[tk])
                            cx.op("dve", lambda e: e.reciprocal(out=p1[:], in_=p1[:]), r=R_, w=[tk])
                            tt(cx, "dve", p2_[:], dd[:], p1[:], ALU.mult, r=R_, w=[tk])
                            tt(cx, "dve", eq1[:], eq1[:], bc3(p1[:], 8), ALU.mult, r=R_, w=[tk])
                            tt(cx, "dve", eq2[:], eq2[:], bc3(p2_[:], 8), ALU.mult, r=R_, w=[tk])
                            tt(cx, "dve", COMB[:], eq1[:], eq2[:], ALU.add, r=R_, w=[tk])
                            for tb in range(4):
                                for tsub in range(4):
                                    ti = tb * 4 + tsub
                                    cx.op("pe", lambda e: e.transpose(out=psL[0:8, tsub * 128:(tsub + 1) * 128], in_=COMB[:, ti, :], identity=ident),
                                          r=[tk, t_consts], w=[t_psL])
                                cp(cx, "act", CT[:, tb * 512:(tb + 1) * 512], psL[0:8, :], r=[t_psL], w=[t_CT])
                            tap("t_CT", CT[:], [t_CT])
                        else:
                            rms_to_h(p2, xT, t_x, hT, t_h, acol[:, l * 2 + 1, :], mods[:, l * 2 + 1, 0:8])
                        cx.barrier()
                    NG = 5
                    stgf = Stager(ph, 3072, 2)
                    G = sb("G", [128, NG, S], BF16, scope=ph); t_G = toks(NG)
                    W13 = [sb(f"W13_{i}", [128, 2048], BF16, scope=ph) for i in range(2)]; t_W13 = toks(2)
                    NW2 = NG + 3
                    W2 = [sb(f"W2_{i}", [128, D], BF16, scope=ph) for i in range(NW2)]; t_W2 = toks(NW2)
                    CB = sb("CB", [128, S], BF16, scope=ph); t_CB = Tok()
                    SA = [sb(f"SA{i}", [128, 512], BF16, scope=ph) for i in range(2)]; t_SA = toks(2)
                    TB = [sb(f"TB{i}", [128, 512], BF16, scope=ph) for i in range(2)]; t_TB = toks(2)
                    psA = [ps(f"fpsA{i}", [128, 512], scope=ph) for i in range(2)]; t_psA = toks(2)
                    psB = [ps(f"fpsB{i}", [128, 512], scope=ph) for i in range(2)]; t_psB = toks(2)
                    psO = [ps(f"fpsO{i}", [128, 512], scope=ph) for i in range(2)]; t_psO = toks(2)
                    g2col = mods[:, l * 2 + 1, 16:24]
                    wi = 0
                    w2i = 0
                    ai = 0
                    oi = 0
                    for e in range(NEXP if moe else 1):
                        pack = moe_in[e] if moe else ffn_in
                        nf = NF_MOE if moe else NF_DENSE
                        if moe:
                            for tb in range(4):
                                cs = slice(tb * 512, (tb + 1) * 512)
                                mm(cx, psO[oi][:], sel[0:8, e, :], CT[:, cs], True, True, r=[t_sel, t_CT], w=[t_psO[oi]])
                                cp(cx, "act", CB[:, cs], psO[oi][:], r=[t_psO[oi]], w=[t_CB])
                                oi = 1 - oi
                        groups = [list(range(i, min(i + NG, nf))) for i in range(0, nf, NG)]
                        for grp in groups:
                            w2s = []
                            for fi, f in enumerate(grp):
                                w13 = W13[wi % 2]; t_w13 = t_W13[wi % 2]
                                wi += 1
                                w2 = W2[w2i % NW2]; t_w2 = t_W2[w2i % NW2]
                                w2i += 1
                                w2s.append((w2, t_w2))
                                sbi = stgf.i % 2
                                stgf.i += 1
                                cx.dma("sp", stgf.bufs[sbi][:], pack[f, :, :], w=[stgf.tk[sbi]])
                                cp(cx, "dve", w13[:], stgf.bufs[sbi][:, 0:2048], r=[stgf.tk[sbi]], w=[t_w13])
                                cp(cx, "act", w2[:], stgf.bufs[sbi][:, 2048:3072], r=[stgf.tk[sbi]], w=[t_w2])
                                for tb in range(4):
                                    cs = slice(tb * 512, (tb + 1) * 512)
                                    hs = slice(1 + tb * 512, 1 + (tb + 1) * 512)
                                    for kc in range(8):
                                        mm(cx, psA[ai][:], w13[:, kc * 128:(kc + 1) * 128], hT[:, kc, hs], kc == 0, kc == 7, r=[t_w13, t_h[kc]], w=[t_psA[ai]])
                                    for kc in range(8):
                                        mm(cx, psB[ai][:], w13[:, 1024 + kc * 128:1024 + (kc + 1) * 128], hT[:, kc, hs], kc == 0, kc == 7, r=[t_w13, t_h[kc]], w=[t_psB[ai]])
                                    act(cx, SA[ai][:], psA[ai][:], AF.Silu, r=[t_psA[ai]], w=[t_SA[ai]])
                                    if moe:
                                        tt(cx, "dve", TB[ai][:], psB[ai][:], CB[:, cs], ALU.mult, r=[t_psB[ai], t_CB], w=[t_TB[ai]])
                                        tt(cx, "pool", G[:, fi, cs], SA[ai][:], TB[ai][:], ALU.mult, r=[t_SA[ai], t_TB[ai]], w=[t_G[fi]])
                                    else:
                                        tt(cx, "dve", G[:, fi, cs], psB[ai][:], SA[ai][:], ALU.mult, r=[t_psB[ai], t_SA[ai]], w=[t_G[fi]])
                                    ai = 1 - ai
                            for mc in range(8):
                                for tb in range(4):
                                    cs = slice(tb * 512, (tb + 1) * 512)
                                    for fi in range(len(grp)):
                                        mm(cx, psO[oi][:], w2s[fi][0][:, mc * 128:(mc + 1) * 128], G[:, fi, cs], fi == 0, fi == len(grp) - 1,
                                           r=[w2s[fi][1], t_G[fi]], w=[t_psO[oi]])
                                    stt(cx, xT[:, mc, cs], psO[oi][:], g2col[:, mc:mc + 1], xT[:, mc, cs], ALU.mult, ALU.add,
                                        r=[t_psO[oi], t_x[mc], t_mods], w=[t_x[mc]])
                                    oi = 1 - oi
                    cx.barrier()
                if stop == f"F_{l}":
                    for kc in range(8):
                        cx.dma("sp", xs_d[kc * 128:(kc + 1) * 128, :], xT[:, kc, :], r=[t_x[kc]], w=[t_xs])
                    cx.barrier()
                    break
            else:
                with ExitStack() as ph:
                    sq = [sb(f"fsq{i}", [128, 512], scope=ph) for i in range(2)]; t_sq = toks(2)
                    rstd = [sb(f"frstd{i}", [128, 512], scope=ph) for i in range(2)]; t_rstd = toks(2)
                    ot = [sb(f"fot{i}", [128, 512], scope=ph) for i in range(4)]; t_ot = toks(4)
                    pss = [ps(f"fpss{i}", [128, 512], scope=ph) for i in range(2)]; t_pss = toks(2)
                    t_out = Tok()
                    k_ = 0
                    for tb in range(4):
                        cs = slice(tb * 512, (tb + 1) * 512)
                        pb_ = tb % 2
                        for kc in range(8):
                            b = k_ % 2
                            k_ += 1
                            act(cx, sq[b][:], xT[:, kc, cs], AF.Square, r=[t_x[kc]], w=[t_sq[b]])
                            mm(cx, pss[pb_][:], ones128, sq[b][:], kc == 0, kc == 7, r=[t_sq[b], t_consts], w=[t_pss[pb_]])
                        act(cx, rstd[pb_][:], pss[pb_][:], AF.Sqrt, r=[t_pss[pb_]], w=[t_rstd[pb_]], bias=NORM_EPS, scale=1.0 / D)
                        cx.op("dve", lambda e: e.reciprocal(out=rstd[pb_][:], in_=rstd[pb_][:]), r=[t_rstd[pb_]], w=[t_rstd[pb_]])
                        for kc in range(8):
                            b = k_ % 4
                            k_ += 1
                            stt(cx, ot[b][:], xT[:, kc, cs], acol[:, NL * 2, kc:kc + 1], rstd[pb_][:], ALU.mult, ALU.mult,
                                r=[t_x[kc], t_rstd[pb_], t_acol], w=[t_ot[b]])
                            cx.dma("sp", out_T[kc * 128:(kc + 1) * 128, cs], ot[b][:], r=[t_ot[b]], w=[t_out])
                    cx.barrier()
            if xT is not None:
                xscope.close()
    except StopBuild:
        pass
    return nc


def _host_layout(inputs):
    f = lambda a: np.ascontiguousarray(a, dtype=np.float32)
    w = {}
    col = lambda v: np.asarray(v, np.float32).reshape(-1, 128).T
    col64 = lambda v: np.pad(np.asarray(v, np.float32).reshape(-1, 64).T, ((0, 64), (0, 0)))
    vecs = np.zeros((128, NV), np.float32)

    def put(name, arr):
        o, n = VEC_COLS[name]
        assert arr.shape == (128, n), (name, arr.shape, n)
        vecs[:, o:o + n] = arr

    for l in range(NL):
        put(f"norm_mix{l}", col(inputs["norm_mix"][l]))
        put(f"norm_ffn{l}", col(inputs["norm_ffn"][l]))
        put(f"ada_b{l}0", col(inputs["ada_b"][l, 0]))
        put(f"ada_b{l}1", col(inputs["ada_b"][l, 1]))
        put(f"mu{l}", col(inputs["tshift_mu"][l]))
        put(f"q_norm{l}", col(inputs["q_norm"][l]))
        put(f"kv_norm{l}", col(inputs["kv_norm"][l]))
        for n in ("w0", "a0", "k_k", "k_a", "lnx_g", "lnx_b"):
            put(f"{n}{l}", col64(inputs[n][l]))
        put(f"r_k{l}", col64(inputs["r_k"][l].reshape(-1)))
    put("norm_final", col(inputs["norm_final"]))
    inv_freq = (10000.0 ** (-np.arange(0, 32, 2, dtype=np.float32) / 32)).astype(np.float32)
    rf = np.zeros((128, 1), np.float32); rs = np.zeros((128, 1), np.float32)
    rf[64:80, 0] = inv_freq; rf[80:96, 0] = inv_freq
    rs[64:80, 0] = -1.0; rs[80:96, 0] = 1.0
    put("ropef", rf); put("ropes", rs)
    put("router_b", np.tile(np.asarray(inputs["router_b"][0], np.float32)[None, :], (128, 1)))
    w["vecs"] = vecs

    consts = np.zeros((128, 5, 128), np.float32)
    consts[:, 0, :] = np.eye(128)
    consts[:, 1, :] = 1.0
    kk = np.arange(128)[:, None] // 64; qq = np.arange(128)[None, :] // 64
    consts[:, 2, :] = (kk <= qq)
    w["consts"] = consts
    c64 = np.zeros((64, 2, 8, 128), np.float32)
    s_ = np.arange(64)[:, None]; t_ = np.arange(64)[None, :]
    mT = np.concatenate([(s_ < t_), (s_ <= t_)], axis=1).astype(np.float32)
    c64[:, 0, :, :] = mT[:, None, :]
    c64[:, 1, :, 0:64] = (s_ > t_).astype(np.float32)[:, None, :]
    c64[:, 1, :, 64:128] = np.eye(64, dtype=np.float32)[:, None, :]
    w["c64"] = c64
    sel = np.zeros((16, 8, 128), np.float32)
    for e in range(8):
        sel[e, e, :] = 1.0
    w["sel"] = sel

    w["adaw"] = f(np.asarray(inputs["ada_w"]).reshape(NL, 2, 8, 128, 3072).transpose(0, 1, 3, 2, 4))
    win = np.asarray(inputs["w_in"], np.float32)
    kr = np.zeros((NL, D, 128), np.float32)
    kr[:, :, 64:96] = win[:, :, 2432:2464]
    winp = np.concatenate([win[:, :, 0:2432], win[:, :, 2464:4512], kr], axis=2)
    w["win"] = f(winp.reshape(NL, 8, 128, NCH // 4, 4, 128).transpose(0, 3, 2, 4, 1, 5))
    w["waup"] = f(np.concatenate([inputs["w_up"], inputs["a_up"]], axis=1))
    w["gup"] = f(inputs["g_up"])
    w["rout"] = f(np.asarray(inputs["rwkv_out"]).reshape(NL, 8, 64, D).transpose(0, 2, 1, 3))
    wqb = np.asarray(inputs["w_qb"], np.float32)
    w["wqb"] = f(wqb.reshape(NL, 3, 128, 768).transpose(0, 2, 1, 3))
    wq4 = wqb.reshape(NL, 384, 8, 96).copy()
    sw = wq4.copy()
    sw[..., 64:80] = wq4[..., 80:96]
    sw[..., 80:96] = wq4[..., 64:80]
    w["wqbs"] = f(sw.reshape(NL, 3, 128, 768).transpose(0, 2, 1, 3))
    wkvb = np.asarray(inputs["w_kvb"], np.float32).reshape(NL, 2, 128, 8, 128)
    w["wkvk"] = f(wkvb[..., 0:64].reshape(NL, 2, 128, 512).transpose(0, 2, 1, 3))
    w["wkvv"] = f(wkvb[..., 64:128].reshape(NL, 2, 128, 512).transpose(0, 2, 1, 3))
    w["mout"] = f(np.asarray(inputs["mla_out"]).reshape(NL, 8, 64, D).transpose(0, 2, 1, 3))
    w["wo"] = f(np.asarray(inputs["w_o"]).reshape(NL, 8, 128, D).transpose(0, 2, 1, 3))

    def pack(w1, w3, w2, nf):
        a = np.asarray(w1, np.float32).reshape(8, 128, nf, 128).transpose(2, 1, 0, 3).reshape(nf, 128, 1024)
        b = np.asarray(w3, np.float32).reshape(8, 128, nf, 128).transpose(2, 1, 0, 3).reshape(nf, 128, 1024)
        c = np.asarray(w2, np.float32).reshape(nf, 128, 1024)
        return np.concatenate([a, b, c], axis=2)

    w["ffnp"] = f(pack(inputs["ffn_w1"][0], inputs["ffn_w3"][0], inputs["ffn_w2"][0], NF_DENSE))
    w["moep"] = f(np.stack([pack(inputs["moe_w1"][0, e], inputs["moe_w3"][0, e], inputs["moe_w2"][0, e], NF_MOE)
                            for e in range(NEXP)]))
    w["routw"] = f(np.asarray(inputs["router_w"][0]).reshape(8, 128, 8).transpose(1, 0, 2))
    return w


def _core_inputs(inputs, b):
    return {
        "xT_in": np.ascontiguousarray(np.asarray(inputs["x"][b], np.float32).T),
        "cT": np.ascontiguousarray(np.asarray(inputs["c"][b], np.float32).reshape(8, 128).T),
        "pos": np.ascontiguousarray(np.tile(np.asarray(inputs["positions"][b], np.int32)[None, :], (32, 1))),
    }


def kernel(**inputs):
    w = _host_layout(inputs)
    nc = build()
    in_maps = []
    for b in range(8):
        m = dict(w)
        m.update(_core_inputs(inputs, b))
        in_maps.append(m)
    res = run_bass_kernel_spmd(nc, in_maps, core_ids=list(range(8)))
    out = np.stack([np.ascontiguousarray(r["outT"].T) for r in res.results], axis=0)
    return out.astype(np.float32)
```

```python
import math
import numpy as np
import concourse.bass as bass
import concourse.mybir as mybir
from concourse.bass_utils import run_bass_kernel_spmd
from contextlib import ExitStack

F32 = mybir.dt.float32
BF16 = mybir.dt.bfloat16
I32 = mybir.dt.int32
ALU = mybir.AluOpType
AF = mybir.ActivationFunctionType
AX = mybir.AxisListType

S = 2048
D = 1024
NL = 2
NCH = 36
DECAY_SCALE = math.exp(-0.5)
LNX_EPS = 64e-5
NORM_EPS = 1e-6
NF_DENSE = 22
NF_MOE = 28
NEXP = 8

VEC_COLS = {}
_off = 0


def _vc(name, n):
    global _off
    VEC_COLS[name] = (_off, n)
    _off += n


for _l in range(NL):
    _vc(f"norm_mix{_l}", 8)
    _vc(f"norm_ffn{_l}", 8)
    _vc(f"ada_b{_l}0", 24)
    _vc(f"ada_b{_l}1", 24)
    _vc(f"mu{_l}", 14)
    _vc(f"q_norm{_l}", 3)
    _vc(f"kv_norm{_l}", 2)
    for _n in ("w0", "a0", "k_k", "k_a", "r_k", "lnx_g", "lnx_b"):
        _vc(f"{_n}{_l}", 8)
_vc("norm_final", 8)
_vc("ropef", 1)
_vc("ropes", 1)
_vc("router_b", 8)
NV = _off


class StopBuild(Exception):
    pass


class Tok:
    __slots__ = ("w", "r")

    def __init__(self):
        self.w = None
        self.r = {}


def toks(n):
    return [Tok() for _ in range(n)]


class Ctx:
    def __init__(self, nc, es):
        self.nc = nc
        self.eng = {"pe": nc.tensor, "act": nc.scalar, "dve": nc.vector, "pool": nc.gpsimd, "sp": nc.sync}
        self.NR = 8
        self.counters = ["pe", "act", "dve", "pool"] + [f"{q}_d{j}" for q in ("sp", "act", "pool") for j in range(self.NR)]
        self.dma_idx = {"sp": 0, "act": 0, "pool": 0}
        self.sem = {c: es.enter_context(nc.semaphore("s_" + c)) for c in self.counters}
        self.cnt = {c: 0 for c in self.counters}
        self.seen = {s: {c: 0 for c in self.counters} for s in self.eng}

    def _wait(self, stream, deps):
        for c, v in deps.items():
            if c == "pe" and stream == "pe":
                continue
            if self.seen[stream][c] < v:
                self.eng[stream].wait_ge(self.sem[c], v)
                self.seen[stream][c] = v

    def _deps(self, counter, r, w):
        deps = {}
        for t in r:
            if t.w is not None and t.w[1] > deps.get(t.w[0], 0):
                deps[t.w[0]] = t.w[1]
        for t in w:
            if t.w is not None and t.w[1] > deps.get(t.w[0], 0):
                deps[t.w[0]] = t.w[1]
            for c, v in t.r.items():
                if c == counter and "_d" not in c:
                    continue
                if v > deps.get(c, 0):
                    deps[c] = v
        return deps

    def _done(self, counter, v, r, w):
        for t in r:
            if t.r.get(counter, 0) < v:
                t.r[counter] = v
        for t in w:
            t.w = (counter, v)
            t.r = {}

    def op(self, stream, fn, r=(), w=()):
        self._wait(stream, self._deps(stream, r, w))
        inst = fn(self.eng[stream])
        self.cnt[stream] += 1
        v = self.cnt[stream]
        inst.then_inc(self.sem[stream], 1)
        self._done(stream, v, r, w)

    def dma(self, stream, out, in_, r=(), w=()):
        counter = f"{stream}_d{self.dma_idx[stream] % self.NR}"
        self.dma_idx[stream] += 1
        if self.cnt[counter] > 0:
            self._wait(stream, {counter: self.cnt[counter]})
        self._wait(stream, self._deps(counter, r, w))
        inst = self.eng[stream].dma_start(out=out, in_=in_)
        self.cnt[counter] += 16
        v = self.cnt[counter]
        inst.then_inc(self.sem[counter], 16)
        self._done(counter, v, r, w)

    def barrier(self):
        for s in self.eng:
            self._wait(s, {c: self.cnt[c] for c in self.counters if self.cnt[c] > 0 and c != s})


def mm(cx, out, lhsT, rhs, start, stop, r=(), w=()):
    cx.op("pe", lambda e: e.matmul(out, lhsT=lhsT, rhs=rhs, start=start, stop=stop), r=r, w=w)


def act(cx, out, in_, func, r=(), w=(), bias=None, scale=None):
    kw = {}
    if bias is not None:
        kw["bias"] = bias
    if scale is not None:
        kw["scale"] = scale
    cx.op("act", lambda e: e.activation(out=out, in_=in_, func=func, **kw), r=r, w=w)


def tt(cx, eng, out, in0, in1, op, r=(), w=()):
    cx.op(eng, lambda e: e.tensor_tensor(out=out, in0=in0, in1=in1, op=op), r=r, w=w)


def ts(cx, eng, out, in0, s1, s2, op0, op1=None, r=(), w=()):
    if op1 is None:
        cx.op(eng, lambda e: e.tensor_scalar(out=out, in0=in0, scalar1=s1, scalar2=None, op0=op0), r=r, w=w)
    else:
        cx.op(eng, lambda e: e.tensor_scalar(out=out, in0=in0, scalar1=s1, scalar2=s2, op0=op0, op1=op1), r=r, w=w)


def stt(cx, out, in0, scalar, in1, op0, op1, r=(), w=()):
    cx.op("dve", lambda e: e.scalar_tensor_tensor(out=out, in0=in0, scalar=scalar, in1=in1, op0=op0, op1=op1), r=r, w=w)


def cp(cx, eng, out, in_, r=(), w=()):
    if eng == "act":
        cx.op("act", lambda e: e.activation(out=out, in_=in_, func=AF.Copy), r=r, w=w)
    else:
        cx.op(eng, lambda e: e.tensor_copy(out=out, in_=in_), r=r, w=w)


def bc3(ap, n):
    p, h = ap.shape
    return ap.unsqueeze(2).to_broadcast([p, h, n])


def build(stop="all", dbg=None):
    nc = bass.Bass("TRN2", target_bir_lowering=False)

    def din(name, shape, dt=F32):
        return nc.dram_tensor(name, list(shape), dt, kind="ExternalInput").ap()

    def dscr(name, shape, dt=F32):
        kind = "ExternalOutput" if (dbg and name in dbg) else "Internal"
        return nc.dram_tensor(name, list(shape), dt, kind=kind).ap()

    xT_in = din("xT_in", [D, S])
    cT_in = din("cT", [128, 8])
    pos_in = din("pos", [32, S], I32)
    vecs_in = din("vecs", [128, NV])
    consts_in = din("consts", [128, 5, 128])
    c64_in = din("c64", [64, 2, 8, 128])
    sel_in = din("sel", [16, 8, 128])
    adaw_in = din("adaw", [NL, 2, 128, 8, 3072])
    win_in = din("win", [NL, NCH // 4, 128, 4, 8, 128])
    waup_in = din("waup", [NL, 128, 512])
    gup_in = din("gup", [NL, 128, 512])
    rout_in = din("rout", [NL, 64, 8, 1024])
    wqb_in = din("wqb", [NL, 128, 3, 768])
    wqbs_in = din("wqbs", [NL, 128, 3, 768])
    wkvk_in = din("wkvk", [NL, 128, 2, 512])
    wkvv_in = din("wkvv", [NL, 128, 2, 512])
    mout_in = din("mout", [NL, 64, 8, 1024])
    wo_in = din("wo", [NL, 128, 8, 1024])
    ffn_in = din("ffnp", [NF_DENSE, 128, 3072])
    moe_in = din("moep", [NEXP, NF_MOE, 128, 3072])
    rw_in = din("routw", [128, 8, 8])
    out_T = nc.dram_tensor("outT", [D, S], F32, kind="ExternalOutput").ap()

    projT = [dscr(f"projT{l}", [NCH * 128, S]) for l in range(NL)]
    yaT_d = [dscr(f"yaT{l}", [512, S], BF16) for l in range(NL)]
    oT_d = [dscr(f"oT{l}", [512, S], BF16) for l in range(NL)]
    xs_d = dscr("xs", [D, S])
    rope_d = dscr("rope", [2, 32, S])

    es = ExitStack()
    try:
        with es:
            cx = Ctx(nc, es)

            uid = [0]

            def sb(name, shape, dt=F32, scope=None):
                uid[0] += 1
                return (scope or es).enter_context(nc.sbuf_tensor(f"sb{uid[0]}_{name}", list(shape), dt))

            def ps(name, shape, dt=F32, scope=None):
                uid[0] += 1
                return (scope or es).enter_context(nc.psum_tensor(f"ps{uid[0]}_{name}", list(shape), dt))

            taps = {}

            def tap(name, ap, rtoks):
                stop_here = dbg is not None and (name + "!") in dbg
                if not dbg or (name not in dbg and not stop_here):
                    return
                shp = list(ap.shape)
                d = nc.dram_tensor(name, shp, F32, kind="ExternalOutput").ap()
                tk = Tok()
                cx.dma("sp", d, ap, r=rtoks, w=[tk])
                cx.barrier()
                if stop_here:
                    raise StopBuild()

            class Stager:
                def __init__(self, scope, ncols, nbuf=2, parts=128):
                    self.bufs = [sb("stg", [parts, ncols], F32, scope=scope) for _ in range(nbuf)]
                    self.tk = toks(nbuf)
                    self.i = 0

                def load(self, dst2d, src2d, eng, w, r=()):
                    p, n = src2d.shape
                    b = self.i % len(self.bufs)
                    self.i += 1
                    cx.dma("sp", self.bufs[b][0:p, 0:n], src2d, r=list(r), w=[self.tk[b]])
                    cp(cx, eng, dst2d, self.bufs[b][0:p, 0:n], r=[self.tk[b]], w=w)

            vecs = sb("vecs", [128, NV]); t_vecs = Tok()
            consts = sb("consts", [128, 5, 128]); t_consts = Tok()
            c64 = sb("c64", [64, 2, 8, 128]); t_c64 = Tok()
            sel = sb("sel", [16, 8, 128]); t_sel = Tok()
            onesb = sb("onesb", [128, 128], BF16); t_onesb = Tok()
            identb = sb("identb", [128, 128], BF16)
            ones64 = sb("ones64", [64, 256]); t_ones64 = Tok()
            mods = sb("mods", [128, NL * 2, 24]); t_mods = Tok()
            acol = sb("acol", [128, NL * 2 + 1, 8]); t_acol = Tok()
            omm = sb("omm", [128, NL, 14]); t_omm = Tok()
            omka = sb("omka", [64, NL, 8]); t_omka = Tok()
            sc = sb("sc", [128, 8]); t_sc = Tok()

            cx.dma("sp", vecs[:], vecs_in[:, :], w=[t_vecs])
            cx.dma("sp", consts[:], consts_in[:, :, :], w=[t_consts])
            cx.dma("sp", c64[:], c64_in[:, :, :, :], w=[t_c64])
            cx.dma("sp", sel[:], sel_in[:, :, :], w=[t_sel])
            cx.dma("sp", sc[:], cT_in[:, :], w=[t_sc])
            ident = consts[:, 0, :]
            ones128 = consts[:, 1, :]
            mlamask = consts[:, 2, :]
            cp(cx, "dve", onesb[:], consts[:, 1, :], r=[t_consts], w=[t_onesb])
            cp(cx, "dve", identb[:], consts[:, 0, :], r=[t_consts], w=[t_onesb])
            cx.op("dve", lambda e: e.memset(ones64[:], 1.0), w=[t_ones64])

            def V(name, rows=128):
                o, n = VEC_COLS[name]
                return vecs[0:rows, o:o + n]

            act(cx, sc[:], sc[:], AF.Silu, r=[t_sc], w=[t_sc])

            with ExitStack() as ph:
                wb = [sb(f"adawb{i}", [128, 8, 512], scope=ph) for i in range(2)]
                t_wb = toks(2)
                pmod = ps("pmod", [128, 512], scope=ph); t_pmod = Tok()
                it = 0
                for l in range(NL):
                    for j in range(2):
                        for g in range(6):
                            b = it % 2
                            it += 1
                            cx.dma("sp", wb[b][:], adaw_in[l, j, :, :, g * 512:(g + 1) * 512], w=[t_wb[b]])
                            for nn in range(4):
                                col = g * 4 + nn
                                for kc in range(8):
                                    mm(cx, pmod[:, col:col + 1], wb[b][:, kc, nn * 128:(nn + 1) * 128], sc[:, kc:kc + 1],
                                       kc == 0, kc == 7, r=[t_wb[b], t_sc], w=[t_pmod])
                        o, n = VEC_COLS[f"ada_b{l}{j}"]
                        tt(cx, "dve", mods[:, l * 2 + j, :], pmod[:, 0:24], vecs[:, o:o + 24], ALU.add,
                           r=[t_pmod, t_vecs], w=[t_mods])
                        nm = V(f"norm_mix{l}") if j == 0 else V(f"norm_ffn{l}")
                        stt(cx, acol[:, l * 2 + j, :], mods[:, l * 2 + j, 8:16], 1.0, nm, ALU.add, ALU.mult,
                            r=[t_mods, t_vecs], w=[t_acol])
                cp(cx, "dve", acol[:, NL * 2, :], V("norm_final"), r=[t_vecs], w=[t_acol])
                for l in range(NL):
                    ts(cx, "dve", omm[:, l, :], V(f"mu{l}"), -1.0, 1.0, ALU.mult, ALU.add, r=[t_vecs], w=[t_omm])
                    ts(cx, "dve", omka[:, l, :], V(f"k_a{l}", 64), -1.0, 1.0, ALU.mult, ALU.add, r=[t_vecs], w=[t_omka])
                cx.barrier()
            t_small = [t_vecs, t_consts, t_c64, t_sel, t_onesb, t_ones64, t_mods, t_acol, t_omm, t_omka]

            def rms_to_h(ph, xT, t_x, hT, t_h, a_ap, shift_ap, h32=None, t_h32=None, tb_hook=None):
                sq = [sb(f"sq{i}", [128, 512], scope=ph) for i in range(2)]; t_sq = toks(2)
                rstd = [sb(f"rstd{i}", [128, 512], scope=ph) for i in range(2)]; t_rstd = toks(2)
                tmp = [sb(f"ntmp{i}", [128, 512], scope=ph) for i in range(2)]; t_tmp = toks(2)
                pss = [ps(f"pss{i}", [128, 512], scope=ph) for i in range(2)]; t_pss = toks(2)
                k = 0
                for tb in range(4):
                    cs = slice(tb * 512, (tb + 1) * 512)
                    pb = tb % 2
                    for kc in range(8):
                        b = k % 2
                        k += 1
                        act(cx, sq[b][:], xT[:, kc, cs], AF.Square, r=[t_x[kc]], w=[t_sq[b]])
                        mm(cx, pss[pb][:], ones128, sq[b][:], kc == 0, kc == 7, r=[t_sq[b], t_consts], w=[t_pss[pb]])
                    act(cx, rstd[pb][:], pss[pb][:], AF.Sqrt, r=[t_pss[pb]], w=[t_rstd[pb]], bias=NORM_EPS, scale=1.0 / D)
                    cx.op("dve", lambda e: e.reciprocal(out=rstd[pb][:], in_=rstd[pb][:]), r=[t_rstd[pb]], w=[t_rstd[pb]])
                    for kc in range(8):
                        b = k % 2
                        k += 1
                        tt(cx, "dve", tmp[b][:], xT[:, kc, cs], rstd[pb][:], ALU.mult, r=[t_x[kc], t_rstd[pb]], w=[t_tmp[b]])
                        if h32 is not None:
                            ts(cx, "pool", h32[:, kc, :], tmp[b][:], a_ap[:, kc:kc + 1], shift_ap[:, kc:kc + 1], ALU.mult, ALU.add,
                               r=[t_tmp[b]] + t_small, w=[t_h32])
                            cp(cx, "act", hT[:, kc, 1 + tb * 512:1 + (tb + 1) * 512], h32[:, kc, :], r=[t_h32], w=[t_h[kc]])
                        else:
                            act(cx, hT[:, kc, 1 + tb * 512:1 + (tb + 1) * 512], tmp[b][:], AF.Identity,
                                r=[t_tmp[b]] + t_small, w=[t_h[kc]], bias=shift_ap[:, kc:kc + 1], scale=a_ap[:, kc:kc + 1])
                    if tb_hook is not None:
                        tb_hook(tb)

            xT = None
            xscope = None
            t_x = toks(8)
            t_rope = Tok()
            for l in range(NL):
                if xT is None:
                    xscope = ExitStack()
                    xT = sb("xT", [128, 8, S], scope=xscope)
                    for kc in range(8):
                        cx.dma("sp", xT[:, kc, :], xT_in[kc * 128:(kc + 1) * 128, :], w=[t_x[kc]])
                with ExitStack() as ph:
                    hT = sb("hT", [128, 8, S + 1], BF16, scope=ph); t_h = toks(8)
                    for kc in range(8):
                        cx.op("pool", lambda e: e.memset(hT[:, kc, 0:1], 0.0), w=[t_h[kc]])
                    with ExitStack() as ph2:
                        rms_to_h(ph2, xT, t_x, hT, t_h, acol[:, l * 2, :], mods[:, l * 2, 0:8])
                        cx.barrier()
                    t_xs = Tok()
                    for kc in range(8):
                        cx.dma("sp", xs_d[kc * 128:(kc + 1) * 128, :], xT[:, kc, :], r=[t_x[kc]], w=[t_xs])
                    with ExitStack() as ph2:
                        wbuf = [sb(f"wbuf{i}", [128, 32, 128], BF16, scope=ph2) for i in range(2)]; t_wbuf = toks(2)
                        stg1 = Stager(ph2, 4096, 2)
                        pp = [ps(f"pp{i}", [128, S], scope=ph2) for i in range(2)]; t_pp = toks(2)
                        Pst = [sb(f"Pst{i}", [128, S + 1], scope=ph2) for i in range(2)]; t_P = toks(2)
                        Tst_ = sb("Ptmp", [128, S], scope=ph2); t_T = Tok()
                        Ost = [sb(f"Ost{i}", [128, S], scope=ph2) for i in range(2)]; t_O = toks(2)
                        for i in range(2):
                            cx.op("pool", lambda e: e.memset(Pst[i][:, 0:1], 0.0), w=[t_P[i]])
                        t_proj = Tok()
                        def load_w(g_):
                            stg1.load(wbuf[g_ % 2][:].rearrange("p a m -> p (a m)"), win_in[l, g_, :, :, :, :].rearrange("p c k m -> p (c k m)"),
                                      "dve" if g_ % 2 == 0 else "act", [t_wbuf[g_ % 2]])
                        load_w(0)
                        for g in range(NCH // 4):
                            wbi = g % 2
                            if g + 1 < NCH // 4:
                                load_w(g + 1)
                            for ci in range(4):
                                m = g * 4 + ci
                                pi = m % 2
                                for tb in range(4):
                                    for kc in range(8):
                                        mm(cx, pp[pi][:, tb * 512:(tb + 1) * 512], wbuf[wbi][:, ci * 8 + kc, :],
                                           hT[:, kc, 1 + tb * 512:1 + (tb + 1) * 512], kc == 0, kc == 7,
                                           r=[t_wbuf[wbi], t_h[kc]], w=[t_pp[pi]])
                                if m < 14:
                                    act(cx, Pst[pi][:, 1:S + 1], pp[pi][:], AF.Copy, r=[t_pp[pi]], w=[t_P[pi]])
                                    o, _ = VEC_COLS[f"mu{l}"]
                                    ts(cx, "dve", Tst_[:], Pst[pi][:, 0:S], vecs[:, o + m:o + m + 1], None, ALU.mult,
                                       r=[t_P[pi], t_vecs], w=[t_T])
                                    stt(cx, Ost[pi][:], Pst[pi][:, 1:S + 1], omm[:, l, m:m + 1], Tst_[:], ALU.mult, ALU.add,
                                        r=[t_P[pi], t_T, t_omm], w=[t_O[pi]])
                                elif 19 <= m < 35:
                                    act(cx, Ost[pi][:], pp[pi][:], AF.Sigmoid, r=[t_pp[pi]], w=[t_O[pi]])
                                else:
                                    act(cx, Ost[pi][:], pp[pi][:], AF.Copy, r=[t_pp[pi]], w=[t_O[pi]])
                                cx.dma("sp", projT[l][m * 128:(m + 1) * 128, :], Ost[pi][:], r=[t_O[pi]], w=[t_proj])
                        cx.barrier()
                xscope.close()
                xT = None
                cx.barrier()
                if stop == f"M1_{l}":
                    break
                with ExitStack() as ph:
                    waup = sb("waup", [128, 512], scope=ph); t_waup = Tok()
                    gup = sb("gup", [128, 512], scope=ph); t_gup = Tok()
                    cx.dma("sp", waup[:], waup_in[l, :, :], w=[t_waup])
                    cx.dma("sp", gup[:], gup_in[l, :, :], w=[t_gup])
                    TS = [sb(f"Tstate{i}", [64, 8, 64], scope=ph) for i in range(2)]; t_TS = toks(2)
                    TSb = [sb(f"TstateB{i}", [64, 8, 64], BF16, scope=ph) for i in range(2)]
                    cx.op("dve", lambda e: e.memset(TS[0][:], 0.0), w=[t_TS[0]])
                    cx.op("dve", lambda e: e.memset(TSb[0][:], 0.0), w=[t_TS[0]])
                    cur = 0
                    BT = 256
                    NCB = BT // 64
                    NP = 2
                    hv = lambda name: vecs[0:64, VEC_COLS[f"{name}{l}"][0]:VEC_COLS[f"{name}{l}"][0] + 8]
                    maskT = c64[:, 0, :, :]
                    maskAB = c64[:, 1, :, 0:64]
                    I8 = c64[:, 1, :, 64:128]
                    id64 = identb[0:64, 0:64]
                    o64 = ones64[:, 0:64]
                    t_yad = Tok()
                    for blk in range(S // BT):
                        c0 = blk * BT
                        with ExitStack() as pb:
                            names = ["R", "K", "V", "KK", "B", "E", "BTl", "KTl", "GA", "BO", "YT"]
                            T = {n: sb(n, [64, 8, BT], BF16 if n in ("BTl", "KTl") else F32, scope=pb) for n in names}
                            k = {n: Tok() for n in names}
                            for n in ("Rb", "Ab", "Bhb", "Khb", "Vb"):
                                T[n] = sb(n, [64, 8, BT], BF16, scope=pb)
                                k[n] = Tok()
                            WA = sb("WA", [128, BT], scope=pb); t_WA = Tok()
                            G_ = sb("G", [128, BT], scope=pb); t_G = Tok()
                            WC = sb("WC", [64, 8, NCB], scope=pb); t_WC = Tok()
                            Vtok = sb("Vtok", [64, NCB, 8, 64], BF16, scope=pb); t_Vtok = toks(NCB)
                            BHtok = sb("BHtok", [64, NCB, 8, 64], BF16, scope=pb); t_BHtok = toks(NCB)
                            KHtok = sb("KHtok", [64, NCB, 8, 64], BF16, scope=pb); t_KHtok = toks(NCB)
                            AbT = sb("AbT", [64, NCB, 8, 128], BF16, scope=pb); t_AbT = toks(NCB)
                            AkT = sb("AkT", [64, NCB, 8, 128], BF16, scope=pb); t_AkT = toks(NCB)
                            MT = sb("MT", [64, NCB, 8, 64], BF16, scope=pb); t_MT = toks(NCB)
                            for n, lo in (("R", 0), ("K", 512), ("V", 1024)):
                                cx.dma("sp", T[n][:], projT[l][lo:lo + 512, c0:c0 + BT].rearrange("(h n) t -> n h t", n=64), w=[k[n]])
                            cx.dma("sp", WA[:], projT[l][1536:1664, c0:c0 + BT], w=[t_WA])
                            cx.dma("sp", G_[:], projT[l][1664:1792, c0:c0 + BT], w=[t_G])
                            with ExitStack() as p2:
                                pA = ps("pA", [64, 8, BT], scope=p2); t_pA = Tok()
                                pB = ps("pB", [64, 8, BT], scope=p2); t_pB = Tok()
                                for n in ("IC", "LW", "CU"):
                                    T[n] = sb(n, [64, 8, BT], scope=p2)
                                    k[n] = Tok()
                                act(cx, WA[0:64, :], WA[0:64, :], AF.Tanh, r=[t_WA], w=[t_WA])
                                act(cx, G_[:], G_[:], AF.Sigmoid, r=[t_G], w=[t_G])
                                for h in range(8):
                                    mm(cx, pA[:, h, :], waup[0:64, h * 64:(h + 1) * 64], WA[0:64, :], True, True, r=[t_waup, t_WA], w=[t_pA])
                                for h in range(8):
                                    act(cx, T["LW"][:, h, :], pA[:, h, :], AF.Sigmoid, r=[t_pA, t_vecs], w=[k["LW"]], bias=hv("w0")[:, h:h + 1])
                                tap("t_LW", T["LW"][:], [k["LW"]])
                                for h in range(8):
                                    mm(cx, pB[:, h, :], waup[64:128, h * 64:(h + 1) * 64], WA[64:128, :], True, True, r=[t_waup, t_WA], w=[t_pB])
                                for h in range(8):
                                    act(cx, T["IC"][:, h, :], pB[:, h, :], AF.Sigmoid, r=[t_pB, t_vecs], w=[k["IC"]], bias=hv("a0")[:, h:h + 1])
                                for h in range(8):
                                    mm(cx, pA[:, h, :], gup[:, h * 64:(h + 1) * 64], G_[:], True, True, r=[t_gup, t_G], w=[t_pA])
                                cp(cx, "act", T["GA"][:], pA[:], r=[t_pA], w=[k["GA"]])
                                tap("t_IC", T["IC"][:], [k["IC"]])
                                tap("t_GA", T["GA"][:], [k["GA"]])
                                tt(cx, "dve", T["KK"][:], T["K"][:], bc3(hv("k_k"), BT), ALU.mult, r=[k["K"], t_vecs], w=[k["KK"]])
                                act(cx, T["E"][:], T["KK"][:], AF.Square, r=[k["KK"]], w=[k["E"]])
                                for h in range(8):
                                    mm(cx, pB[:, h, :], o64, T["E"][:, h, :], True, True, r=[k["E"], t_ones64], w=[t_pB])
                                act(cx, T["E"][:], pB[:], AF.Ln, r=[t_pB], w=[k["E"]], bias=1e-12)
                                act(cx, T["E"][:], T["E"][:], AF.Exp, r=[k["E"]], w=[k["E"]], scale=-0.5)
                                tt(cx, "dve", T["KK"][:], T["KK"][:], T["E"][:], ALU.mult, r=[k["KK"], k["E"]], w=[k["KK"]])
                                tt(cx, "dve", T["E"][:], T["IC"][:], bc3(hv("k_a"), BT), ALU.mult, r=[k["IC"], t_vecs], w=[k["E"]])
                                tt(cx, "dve", T["E"][:], T["E"][:], bc3(omka[:, l, :], BT), ALU.add, r=[k["E"], t_omka], w=[k["E"]])
                                tt(cx, "dve", T["K"][:], T["K"][:], T["E"][:], ALU.mult, r=[k["K"], k["E"]], w=[k["K"]])
                                tt(cx, "pool", T["B"][:], T["KK"][:], T["IC"][:], ALU.mult, r=[k["KK"], k["IC"]], w=[k["B"]])
                                tt(cx, "dve", T["E"][:], T["R"][:], bc3(hv("r_k"), BT), ALU.mult, r=[k["R"], t_vecs], w=[k["E"]])
                                tt(cx, "dve", T["E"][:], T["E"][:], T["K"][:], ALU.mult, r=[k["E"], k["K"]], w=[k["E"]])
                                for h in range(8):
                                    mm(cx, pA[:, h, :], o64, T["E"][:, h, :], True, True, r=[k["E"], t_ones64], w=[t_pA])
                                tt(cx, "dve", T["BO"][:], pA[:], T["V"][:], ALU.mult, r=[t_pA, k["V"]], w=[k["BO"]])
                                tap("t_KKn", T["KK"][:], [k["KK"]])
                                tap("t_Kp", T["K"][:], [k["K"]])
                                tap("t_BO", T["BO"][:], [k["BO"]])
                                for h in range(8):
                                    for c in range(NCB):
                                        cs = slice(c * 64, (c + 1) * 64)
                                        cx.op("dve", lambda e: e.tensor_tensor_scan(out=T["CU"][:, h, cs], data0=o64, data1=T["LW"][:, h, cs],
                                                                                    initial=0.0, op0=ALU.mult, op1=ALU.add),
                                              r=[k["LW"], t_ones64], w=[k["CU"]])
                                act(cx, T["E"][:], T["CU"][:], AF.Exp, r=[k["CU"]], w=[k["E"]], scale=-DECAY_SCALE)
                                tt(cx, "dve", T["Rb"][:], T["R"][:], T["E"][:], ALU.mult, r=[k["R"], k["E"]], w=[k["Rb"]])
                                cp(cx, "dve", WC[:], T["E"][:].rearrange("p h (c t) -> p h c t", t=64)[:, :, :, 63], r=[k["E"]], w=[t_WC])
                                tt(cx, "dve", T["E"][:], T["CU"][:], T["LW"][:], ALU.subtract, r=[k["CU"], k["LW"]], w=[k["E"]])
                                act(cx, T["E"][:], T["E"][:], AF.Exp, r=[k["E"]], w=[k["E"]], scale=-DECAY_SCALE)
                                stt(cx, T["Ab"][:], T["KK"][:], -1.0, T["E"][:], ALU.mult, ALU.mult, r=[k["KK"], k["E"]], w=[k["Ab"]])
                                act(cx, T["E"][:], T["CU"][:], AF.Exp, r=[k["CU"]], w=[k["E"]], scale=DECAY_SCALE)
                                tt(cx, "dve", T["BTl"][:], T["B"][:], T["E"][:], ALU.mult, r=[k["B"], k["E"]], w=[k["BTl"]])
                                tt(cx, "dve", T["KTl"][:], T["K"][:], T["E"][:], ALU.mult, r=[k["K"], k["E"]], w=[k["KTl"]])
                                CUv = T["CU"][:].rearrange("p h (c t) -> p (h c) t", t=64)
                                Ev = T["E"][:].rearrange("p h (c t) -> p (h c) t", t=64)
                                tt(cx, "dve", Ev, CUv[:, :, 63:64].to_broadcast([64, 8 * NCB, 64]), CUv, ALU.subtract, r=[k["CU"]], w=[k["E"]])
                                act(cx, T["E"][:], T["E"][:], AF.Exp, r=[k["E"]], w=[k["E"]], scale=-DECAY_SCALE)
                                tt(cx, "dve", T["Bhb"][:], T["B"][:], T["E"][:], ALU.mult, r=[k["B"], k["E"]], w=[k["Bhb"]])
                                tt(cx, "pool", T["Khb"][:], T["K"][:], T["E"][:], ALU.mult, r=[k["K"], k["E"]], w=[k["Khb"]])
                                cp(cx, "act", T["Vb"][:], T["V"][:], r=[k["V"]], w=[k["Vb"]])
                                tap("t_CU", T["CU"][:], [k["CU"]])
                                tap("t_Rt", T["R"][:], [k["R"]])
                                tap("t_At", T["KK"][:], [k["KK"]])
                                tap("t_Bt", T["BTl"][:], [k["BTl"]])
                                tap("t_Kt", T["KTl"][:], [k["KTl"]])
                                tap("t_Bh", T["B"][:], [k["B"]])
                                tap("t_Kh", T["K"][:], [k["K"]])
                                tap("t_WC", WC[:], [t_WC])
                                cx.barrier()
                            with ExitStack() as p2:
                                pT_ = ps("pT", [64, 8, 64], BF16, scope=p2); t_pT = Tok()
                                pG = [ps(f"pG{i}", [64, NP, 8, 64], scope=p2) for i in range(3)]; t_pG = toks(3)
                                pAb = pG[1][:].rearrange("p c h t -> p (c h t)").rearrange("p (h x) -> p h x", x=128)
                                pAk = pG[2][:].rearrange("p c h t -> p (c h t)").rearrange("p (h x) -> p h x", x=128)
                                Pm = [sb(f"Pm{i}", [64, NP, 8, 64], BF16, scope=p2) for i in range(2)]; t_Pm = toks(2)
                                PTm = [sb(f"PTm{i}", [64, NP, 8, 64], BF16, scope=p2) for i in range(2)]; t_PTm = toks(2)
                                IPm = sb("IPm", [64, NP, 8, 64], BF16, scope=p2); t_IPm = Tok()
                                Rm = [sb(f"Rm{i}", [64, NP, 8, 64], BF16, scope=p2) for i in range(2)]; t_Rm = toks(2)
                                Xsb = sb("Xsb", [64, 8, 64], BF16, scope=p2); t_Xsb = Tok()
                                Usb = sb("Usb", [64, 8, 64], BF16, scope=p2); t_Usb = Tok()
                                Ttmp = sb("Ttmp", [64, 8, 64], scope=p2); t_Ttmp = Tok()
                                maskAB4 = maskAB.unsqueeze(1).to_broadcast([64, NP, 8, 64])
                                I84 = I8.unsqueeze(1).to_broadcast([64, NP, 8, 64])
                                for pr in range(NCB // NP):
                                    for c2 in range(NP):
                                        c = pr * NP + c2
                                        cs = slice(c * 64, (c + 1) * 64)
                                        for (src, dst, tdst) in (("Vb", Vtok, t_Vtok), ("Bhb", BHtok, t_BHtok), ("Khb", KHtok, t_KHtok)):
                                            for h in range(8):
                                                cx.op("pe", lambda e: e.transpose(out=pT_[:, h, :], in_=T[src][:, h, cs], identity=id64),
                                                      r=[k[src], t_consts], w=[t_pT])
                                            cp(cx, "act", dst[:, c, :, :], pT_[:], r=[t_pT], w=[tdst[c]])
                                        for h in range(8):
                                            mm(cx, pAb[:, h, 0:64], T["BTl"][:, h, cs], T["Ab"][:, h, cs], True, True, r=[k["BTl"], k["Ab"]], w=[t_pG[1]])
                                            mm(cx, pAb[:, h, 64:128], T["BTl"][:, h, cs], T["Rb"][:, h, cs], True, True, r=[k["BTl"], k["Rb"]], w=[t_pG[1]])
                                            mm(cx, pAk[:, h, 0:64], T["KTl"][:, h, cs], T["Ab"][:, h, cs], True, True, r=[k["KTl"], k["Ab"]], w=[t_pG[2]])
                                            mm(cx, pAk[:, h, 64:128], T["KTl"][:, h, cs], T["Rb"][:, h, cs], True, True, r=[k["KTl"], k["Rb"]], w=[t_pG[2]])
                                            mm(cx, pG[0][:, c2, h, :], T["Ab"][:, h, cs], T["BTl"][:, h, cs], True, True, r=[k["BTl"], k["Ab"]], w=[t_pG[0]])
                                        tt(cx, "dve", AbT[:, c, :, :], pAb, maskT, ALU.mult, r=[t_pG[1], t_c64], w=[t_AbT[c]])
                                        tt(cx, "dve", AkT[:, c, :, :], pAk, maskT, ALU.mult, r=[t_pG[2], t_c64], w=[t_AkT[c]])
                                    tt(cx, "dve", Pm[0][:], pG[0][:], maskAB4, ALU.mult, r=[t_pG[0], t_c64], w=[t_Pm[0]])
                                    cp(cx, "act", PTm[0][:], AbT[:, pr * NP:(pr + 1) * NP, :, 0:64], r=t_AbT[pr * NP:(pr + 1) * NP], w=[t_PTm[0]])
                                    tt(cx, "dve", Rm[0][:], AbT[:, pr * NP:(pr + 1) * NP, :, 0:64], I84, ALU.add, r=t_AbT[pr * NP:(pr + 1) * NP] + [t_c64], w=[t_Rm[0]])

                                    def mmP(pi_):
                                        for c in range(NP):
                                            for h in range(8):
                                                mm(cx, pG[0][:, c, h, :], PTm[pi_][:, c, h, :], Pm[pi_][:, c, h, :], True, True, r=[t_PTm[pi_], t_Pm[pi_]], w=[t_pG[0]])

                                    def mmPT(pi_):
                                        for c in range(NP):
                                            for h in range(8):
                                                mm(cx, pG[1][:, c, h, :], Pm[pi_][:, c, h, :], PTm[pi_][:, c, h, :], True, True, r=[t_PTm[pi_], t_Pm[pi_]], w=[t_pG[1]])

                                    def mmR(ri_):
                                        for c in range(NP):
                                            for h in range(8):
                                                mm(cx, pG[2][:, c, h, :], IPm[:, c, h, :], Rm[ri_][:, c, h, :], True, True, r=[t_IPm, t_Rm[ri_]], w=[t_pG[2]])

                                    pi = 0
                                    ri = 0
                                    for kk_ in range(1, 6):
                                        ni = 1 - pi
                                        mmP(pi)
                                        if kk_ < 5:
                                            mmPT(pi)
                                        if kk_ > 1:
                                            mmR(ri)
                                        if kk_ < 5:
                                            cp(cx, "act", PTm[ni][:], pG[1][:], r=[t_pG[1]], w=[t_PTm[ni]])
                                        cp(cx, "act", Pm[ni][:], pG[0][:], r=[t_pG[0]], w=[t_Pm[ni]])
                                        if kk_ > 1:
                                            cp(cx, "dve", Rm[1 - ri][:], pG[2][:], r=[t_pG[2]], w=[t_Rm[1 - ri]])
                                            ri = 1 - ri
                                        tt(cx, "dve", IPm[:], Pm[ni][:], I84, ALU.add, r=[t_Pm[ni], t_c64], w=[t_IPm])
                                        pi = ni
                                    mmR(ri)
                                    cp(cx, "dve", MT[:, pr * NP:(pr + 1) * NP, :, :], pG[2][:], r=[t_pG[2]], w=t_MT[pr * NP:(pr + 1) * NP])
                                for c in range(NCB):
                                    cs = slice(c * 64, (c + 1) * 64)
                                    To, Tn = TS[cur], TS[1 - cur]
                                    ToB, TnB = TSb[cur], TSb[1 - cur]
                                    t_To, t_Tn = t_TS[cur], t_TS[1 - cur]
                                    pX = pG[0][:, 0, :, :]
                                    pU = pG[0][:, 1, :, :]
                                    pTn = pG[1][:, 0, :, :]
                                    pY = pG[2][:, 0, :, :]
                                    for h in range(8):
                                        mm(cx, pX[:, h, :], T["Ab"][:, h, cs], ToB[:, h, :], True, False, r=[k["Ab"], t_To], w=[t_pG[0]])
                                        mm(cx, pX[:, h, :], AkT[:, c, h, 0:64], Vtok[:, c, h, :], False, True, r=[t_AkT[c], t_Vtok[c]], w=[t_pG[0]])
                                    cp(cx, "act", Xsb[:], pX, r=[t_pG[0]], w=[t_Xsb])
                                    for h in range(8):
                                        mm(cx, pU[:, h, :], MT[:, c, h, :], Xsb[:, h, :], True, True, r=[t_MT[c], t_Xsb], w=[t_pG[0]])
                                    cp(cx, "act", Usb[:], pU, r=[t_pG[0]], w=[t_Usb])
                                    for h in range(8):
                                        mm(cx, pTn[:, h, :], BHtok[:, c, h, :], Usb[:, h, :], True, False, r=[t_BHtok[c], t_Usb], w=[t_pG[1]])
                                        mm(cx, pTn[:, h, :], KHtok[:, c, h, :], Vtok[:, c, h, :], False, True, r=[t_KHtok[c], t_Vtok[c]], w=[t_pG[1]])
                                    tt(cx, "dve", Ttmp[:], To[:], bc3(WC[:, :, c], 64), ALU.mult, r=[t_To, t_WC], w=[t_Ttmp])
                                    tt(cx, "dve", TnB[:], Ttmp[:], pTn, ALU.add, r=[t_Ttmp, t_pG[1]], w=[t_Tn])
                                    tt(cx, "dve", Tn[:], Ttmp[:], pTn, ALU.add, r=[t_Ttmp, t_pG[1], t_Tn], w=[t_Tn])
                                    for h in range(8):
                                        mm(cx, pY[:, h, :], ToB[:, h, :], T["Rb"][:, h, cs], True, False, r=[k["Rb"], t_To], w=[t_pG[2]])
                                        mm(cx, pY[:, h, :], Usb[:, h, :], AbT[:, c, h, 64:128], False, False, r=[t_AbT[c], t_Usb], w=[t_pG[2]])
                                        mm(cx, pY[:, h, :], Vtok[:, c, h, :], AkT[:, c, h, 64:128], False, True, r=[t_AkT[c], t_Vtok[c]], w=[t_pG[2]])
                                    cp(cx, "act", T["YT"][:, :, cs], pY, r=[t_pG[2]], w=[k["YT"]])
                                    cur = 1 - cur
                                cx.barrier()
                            with ExitStack() as p2:
                                pA = ps("pA2", [64, 8, BT], scope=p2); t_pA = Tok()
                                for h in range(8):
                                    mm(cx, pA[:, h, :], o64, T["YT"][:, h, :], True, True, r=[k["YT"], t_ones64], w=[t_pA])
                                stt(cx, T["YT"][:], pA[:], -1.0 / 64, T["YT"][:], ALU.mult, ALU.add, r=[t_pA, k["YT"]], w=[k["YT"]])
                                act(cx, T["E"][:], T["YT"][:], AF.Square, r=[k["YT"]], w=[k["E"]])
                                for h in range(8):
                                    mm(cx, pA[:, h, :], o64, T["E"][:, h, :], True, True, r=[k["E"], t_ones64], w=[t_pA])
                                act(cx, T["E"][:], pA[:], AF.Ln, r=[t_pA], w=[k["E"]], bias=LNX_EPS, scale=1.0 / 64)
                                act(cx, T["E"][:], T["E"][:], AF.Exp, r=[k["E"]], w=[k["E"]], scale=-0.5)
                                tt(cx, "dve", T["YT"][:], T["YT"][:], T["E"][:], ALU.mult, r=[k["YT"], k["E"]], w=[k["YT"]])
                                tt(cx, "dve", T["YT"][:], T["YT"][:], bc3(hv("lnx_g"), BT), ALU.mult, r=[k["YT"], t_vecs], w=[k["YT"]])
                                tt(cx, "dve", T["YT"][:], T["YT"][:], bc3(hv("lnx_b"), BT), ALU.add, r=[k["YT"], t_vecs], w=[k["YT"]])
                                tt(cx, "dve", T["YT"][:], T["YT"][:], T["BO"][:], ALU.add, r=[k["YT"], k["BO"]], w=[k["YT"]])
                                tt(cx, "dve", T["Rb"][:], T["YT"][:], T["GA"][:], ALU.mult, r=[k["YT"], k["GA"], k["Rb"]], w=[k["Rb"]])
                                cx.dma("sp", yaT_d[l][:, c0:c0 + BT].rearrange("(h n) t -> n h t", n=64), T["Rb"][:], r=[k["Rb"]], w=[t_yad])
                                cx.barrier()
                    cx.barrier()
                if stop == f"M2_{l}":
                    break
                with ExitStack() as ph:
                    rr = slice(64, 96)
                    CC = sb("CC", [96, S], scope=ph); t_CC = Tok()
                    SS = sb("SS", [96, S], scope=ph); t_SS = Tok()
                    ropef = vecs[:, VEC_COLS["ropef"][0]:VEC_COLS["ropef"][0] + 1]
                    ropes = vecs[:, VEC_COLS["ropes"][0]:VEC_COLS["ropes"][0] + 1]
                    if l == 0:
                        with ExitStack() as p2:
                            posi = sb("posi", [96, S], I32, scope=p2); t_posi = Tok()
                            ang = sb("ang", [96, S], scope=p2); t_ang = Tok()
                            tq = sb("tq", [96, S], scope=p2); t_tq = Tok()
                            cx.dma("sp", posi[rr, :], pos_in[:, :], w=[t_posi])
                            cp(cx, "dve", ang[rr, :], posi[rr, :], r=[t_posi], w=[t_ang])
                            ts(cx, "dve", ang[rr, :], ang[rr, :], ropef[rr, :], None, ALU.mult, r=[t_ang, t_vecs], w=[t_ang])
                            MAGIC = 12582912.0
                            C1 = 6.28125
                            C2 = 2.0 * math.pi - 6.28125
                            for (dst, tdst, phase) in ((SS, t_SS, 0.0), (CC, t_CC, math.pi / 2)):
                                ts(cx, "dve", tq[rr, :], ang[rr, :], phase, 1.0 / (2.0 * math.pi), ALU.add, ALU.mult, r=[t_ang], w=[t_tq])
                                ts(cx, "dve", tq[rr, :], tq[rr, :], MAGIC, None, ALU.add, r=[t_tq], w=[t_tq])
                                ts(cx, "dve", tq[rr, :], tq[rr, :], -MAGIC, None, ALU.add, r=[t_tq], w=[t_tq])
                                stt(cx, dst[rr, :], tq[rr, :], -C1, ang[rr, :], ALU.mult, ALU.add, r=[t_tq, t_ang], w=[tdst])
                                stt(cx, dst[rr, :], tq[rr, :], -C2, dst[rr, :], ALU.mult, ALU.add, r=[t_tq, tdst], w=[tdst])
                                ts(cx, "dve", dst[rr, :], dst[rr, :], phase, 3.1415925, ALU.add, ALU.min, r=[tdst], w=[tdst])
                                ts(cx, "dve", dst[rr, :], dst[rr, :], -3.1415925, None, ALU.max, r=[tdst], w=[tdst])
                                act(cx, dst[rr, :], dst[rr, :], AF.Sin, r=[tdst], w=[tdst])
                            ts(cx, "dve", SS[rr, :], SS[rr, :], ropes[rr, :], None, ALU.mult, r=[t_SS, t_vecs], w=[t_SS])
                            cx.dma("sp", rope_d[0, :, :], CC[rr, :], r=[t_CC], w=[t_rope])
                            cx.dma("sp", rope_d[1, :, :], SS[rr, :], r=[t_SS], w=[t_rope])
                            cx.barrier()
                    else:
                        cx.dma("sp", CC[rr, :], rope_d[0, :, :], r=[t_rope], w=[t_CC])
                        cx.dma("sp", SS[rr, :], rope_d[1, :, :], r=[t_rope], w=[t_SS])
                    QN = sb("QN", [128, 3, S], BF16, scope=ph); t_QN = Tok()
                    KVN = sb("KVN", [128, 2, S], BF16, scope=ph); t_KVN = Tok()
                    KPE = sb("KPE", [96, S], BF16, scope=ph); t_KPE = Tok()
                    VT = sb("VT", [128, 16, 512], BF16, scope=ph); t_VT = Tok()
                    wqb = sb("wqb", [128, 3, 768], BF16, scope=ph); t_wqb = Tok()
                    wqbs = sb("wqbs", [128, 3, 768], BF16, scope=ph); t_wqbs = Tok()
                    wkvk = sb("wkvk", [128, 2, 512], BF16, scope=ph); t_wkvk = Tok()
                    wkvv = sb("wkvv", [128, 2, 512], BF16, scope=ph); t_wkvv = Tok()
                    cx.dma("pool", wqb[:], wqb_in[l, :, :, :], w=[t_wqb])
                    cx.dma("pool", wqbs[:], wqbs_in[l, :, :, :], w=[t_wqbs])
                    cx.dma("pool", wkvk[:], wkvk_in[l, :, :, :], w=[t_wkvk])
                    cx.dma("pool", wkvv[:], wkvv_in[l, :, :, :], w=[t_wkvv])
                    with ExitStack() as p2:
                        raw = sb("raw", [128, 3, 512], scope=p2); t_raw = Tok()
                        sq = sb("sqm", [128, 512], scope=p2); t_sq = Tok()
                        rs_ = sb("rsm", [128, 512], scope=p2); t_rs = Tok()
                        tmpm = sb("tmpm", [128, 512], scope=p2); t_tmpm = Tok()
                        pss = ps("pssm", [128, 512], scope=p2); t_pss = Tok()
                        for (dst, tdst, nchk, row0, gname) in ((QN, t_QN, 3, 1792, "q_norm"), (KVN, t_KVN, 2, 2176, "kv_norm")):
                            go = VEC_COLS[f"{gname}{l}"][0]
                            for tb in range(4):
                                cs = slice(tb * 512, (tb + 1) * 512)
                                cx.dma("sp", raw[:, 0:nchk, :], projT[l][row0:row0 + nchk * 128, cs].rearrange("(c p) t -> p c t", p=128), w=[t_raw])
                                for c in range(nchk):
                                    act(cx, sq[:], raw[:, c, :], AF.Square, r=[t_raw], w=[t_sq])
                                    mm(cx, pss[:], ones128, sq[:], c == 0, c == nchk - 1, r=[t_sq, t_consts], w=[t_pss])
                                act(cx, rs_[:], pss[:], AF.Sqrt, r=[t_pss], w=[t_rs], bias=NORM_EPS, scale=1.0 / (nchk * 128))
                                cx.op("dve", lambda e: e.reciprocal(out=rs_[:], in_=rs_[:]), r=[t_rs], w=[t_rs])
                                for c in range(nchk):
                                    tt(cx, "dve", tmpm[:], raw[:, c, :], rs_[:], ALU.mult, r=[t_raw, t_rs], w=[t_tmpm])
                                    act(cx, dst[:, c, cs], tmpm[:], AF.Copy, r=[t_tmpm, t_vecs], w=[tdst], scale=vecs[:, go + c:go + c + 1])
                        KR = sb("KR", [96, S], scope=p2); t_KR = Tok()
                        KRs = sb("KRs", [96, S], scope=p2); t_KRs = Tok()
                        kb = 35 * 128
                        cx.dma("sp", KR[64:96, :], projT[l][kb + 64:kb + 96, :], w=[t_KR])
                        cx.dma("sp", KRs[64:80, :], projT[l][kb + 80:kb + 96, :], w=[t_KRs])
                        cx.dma("sp", KRs[80:96, :], projT[l][kb + 64:kb + 80, :], w=[t_KRs])
                        tt(cx, "dve", KR[rr, :], KR[rr, :], CC[rr, :], ALU.mult, r=[t_KR, t_CC], w=[t_KR])
                        tt(cx, "dve", KRs[rr, :], KRs[rr, :], SS[rr, :], ALU.mult, r=[t_KRs, t_SS], w=[t_KRs])
                        tt(cx, "dve", KPE[rr, :], KR[rr, :], KRs[rr, :], ALU.add, r=[t_KR, t_KRs], w=[t_KPE])
                        pV = ps("pV", [128, 512], scope=p2); t_pV = Tok()
                        for tt_ in range(16):
                            for kc in range(2):
                                mm(cx, pV[:], KVN[:, kc, tt_ * 128:(tt_ + 1) * 128], wkvv[:, kc, :], kc == 0, kc == 1, r=[t_KVN, t_wkvv], w=[t_pV])
                            cp(cx, "act", VT[:, tt_, :], pV[:], r=[t_pV], w=[t_VT])
                        cx.barrier()
                    with ExitStack() as p2:
                        QH = [sb(f"QH{i}", [96, S], BF16, scope=p2) for i in range(2)]; t_QH = toks(2)
                        KH = [sb(f"KH{i}", [96, S], BF16, scope=p2) for i in range(2)]; t_KH = toks(2)
                        PT = [sb(f"PT{i}", [128, 16, 128], BF16, scope=p2) for i in range(2)]; t_PT = toks(2)
                        T1 = sb("T1", [96, 512], scope=p2); t_T1 = Tok()
                        T2 = sb("T2", [96, 512], scope=p2); t_T2 = Tok()
                        OTh = [sb(f"OTh{i}", [64, S], BF16, scope=p2) for i in range(2)]; t_OTh = toks(2)
                        RD = sb("RD", [64, 128], scope=p2); t_RD = Tok()
                        mlam = sb("mlam", [128, 128], BF16, scope=p2); t_mlam = Tok()
                        cp(cx, "dve", mlam[:], mlamask, r=[t_consts], w=[t_mlam])
                        psQ = ps("psQ", [96, 512], scope=p2); t_psQ = Tok()
                        psQs = ps("psQs", [96, 512], scope=p2); t_psQs = Tok()
                        psK = ps("psK", [64, 512], scope=p2); t_psK = Tok()
                        psS = [ps(f"psS{i}", [128, 4, 128], scope=p2) for i in range(2)]; t_psS = toks(2)
                        psOD = [ps(f"psOD{i}", [64, 2, 128], scope=p2) for i in range(2)]; t_psOD = toks(2)
                        t_od = Tok()
                        si_box = [0]
                        def proj_tb(h, tb):
                            hb = h % 2
                            cs = slice(tb * 512, (tb + 1) * 512)
                            for kc in range(3):
                                mm(cx, psQ[:], wqb[:, kc, h * 96:(h + 1) * 96], QN[:, kc, cs], kc == 0, kc == 2, r=[t_wqb, t_QN], w=[t_psQ])
                            for kc in range(3):
                                mm(cx, psQs[:], wqbs[:, kc, h * 96:(h + 1) * 96], QN[:, kc, cs], kc == 0, kc == 2, r=[t_wqbs, t_QN], w=[t_psQs])
                            for kc in range(2):
                                mm(cx, psK[:], wkvk[:, kc, h * 64:(h + 1) * 64], KVN[:, kc, cs], kc == 0, kc == 1, r=[t_wkvk, t_KVN], w=[t_psK])
                            tt(cx, "dve", T1[rr, :], psQ[rr, :], CC[rr, cs], ALU.mult, r=[t_psQ, t_CC], w=[t_T1])
                            tt(cx, "dve", T2[rr, :], psQs[rr, :], SS[rr, cs], ALU.mult, r=[t_psQs, t_SS], w=[t_T2])
                            cp(cx, "act", QH[hb][0:64, cs], psQ[0:64, :], r=[t_psQ, t_T1], w=[t_QH[hb]])
                            tt(cx, "pool", QH[hb][rr, cs], T1[rr, :], T2[rr, :], ALU.add, r=[t_T1, t_T2], w=[t_QH[hb]])
                            cp(cx, "act", KH[hb][0:64, cs], psK[:], r=[t_psK], w=[t_KH[hb]])
                        def proj_fin(h):
                            hb = h % 2
                            cp(cx, "pool", KH[hb][rr, :], KPE[rr, :], r=[t_KPE], w=[t_KH[hb]])
                        for tb in range(4):
                            proj_tb(0, tb)
                        proj_fin(0)
                        for h in range(8):
                            hb = h % 2
                            def s_part(qb, h=h, hb=hb):
                                nk = qb + 1
                                qs = slice(qb * 128, (qb + 1) * 128)
                                pt = PT[qb % 2]; t_pt = t_PT[qb % 2]
                                for g in range((nk + 3) // 4):
                                    pS = psS[si_box[0]]; t_pS = t_psS[si_box[0]]
                                    si_box[0] = 1 - si_box[0]
                                    n_in = min(4, nk - g * 4)
                                    for j in range(n_in):
                                        kt = g * 4 + j
                                        mm(cx, pS[:, j, :], KH[hb][:, kt * 128:(kt + 1) * 128], QH[hb][:, qs], True, True, r=[t_KH[hb], t_QH[hb]], w=[t_pS])
                                    act(cx, pt[:, g * 4:g * 4 + n_in, :], pS[:, 0:n_in, :], AF.Exp, r=[t_pS], w=[t_pt], scale=96.0 ** -0.5)
                                tt(cx, "pool", pt[:, qb, :], pt[:, qb, :], mlam[:], ALU.mult, r=[t_pt, t_mlam], w=[t_pt])
                            def od_part(qb, h=h, hb=hb):
                                nk = qb + 1
                                qs = slice(qb * 128, (qb + 1) * 128)
                                pt = PT[qb % 2]; t_pt = t_PT[qb % 2]
                                pOD = psOD[qb % 2]; t_pOD = t_psOD[qb % 2]
                                for kt in range(nk):
                                    mm(cx, pOD[:, 0, :], VT[:, kt, h * 64:(h + 1) * 64], pt[:, kt, :], kt == 0, kt == nk - 1, r=[t_VT, t_pt], w=[t_pOD])
                                for kt in range(nk):
                                    mm(cx, pOD[:, 1, :], onesb[:, 0:64], pt[:, kt, :], kt == 0, kt == nk - 1, r=[t_onesb, t_pt], w=[t_pOD])
                                cx.op("dve", lambda e: e.reciprocal(out=RD[:], in_=pOD[:, 1, :]), r=[t_pOD], w=[t_RD])
                                tt(cx, "dve", OTh[hb][:, qs], pOD[:, 0, :], RD[:], ALU.mult, r=[t_pOD, t_RD], w=[t_OTh[hb]])
                            s_part(0)
                            for qb in range(16):
                                if qb + 1 < 16:
                                    s_part(qb + 1)
                                od_part(qb)
                                if h + 1 < 8 and qb in (2, 5, 8, 11):
                                    proj_tb(h + 1, (2, 5, 8, 11).index(qb))
                                if h + 1 < 8 and qb == 12:
                                    proj_fin(h + 1)
                            cx.dma("sp", oT_d[l][h * 64:(h + 1) * 64, :], OTh[hb][:], r=[t_OTh[hb]], w=[t_od])
                        cx.barrier()
                if stop == f"M3_{l}":
                    break
                xscope = ExitStack()
                xT = sb("xT", [128, 8, S], scope=xscope)
                for kc in range(8):
                    cx.dma("sp", xT[:, kc, :], xs_d[kc * 128:(kc + 1) * 128, :], r=[t_xs], w=[t_x[kc]])
                with ExitStack() as ph:
                    ROUT = sb("ROUT", [64, 8, D], BF16, scope=ph); t_ROUT = Tok()
                    MOUT = sb("MOUT", [64, 8, D], BF16, scope=ph); t_MOUT = Tok()
                    WO = sb("WO", [128, 8, D], BF16, scope=ph); t_WO = Tok()
                    YAs = [sb(f"YA{i}", [64, 8, 512], BF16, scope=ph) for i in range(2)]; t_YAs = toks(2)
                    OTbs = [sb(f"OTb{i}", [64, 8, 512], BF16, scope=ph) for i in range(2)]; t_OTbs = toks(2)
                    stg4 = Stager(ph, 2048, 2)
                    for pc in range(4):
                        stg4.load(ROUT[:, pc * 2:(pc + 1) * 2, :].rearrange("p a m -> p (a m)"),
                                  rout_in[l, :, pc * 2:(pc + 1) * 2, :].rearrange("p a m -> p (a m)"), "dve", [t_ROUT])
                        stg4.load(MOUT[:, pc * 2:(pc + 1) * 2, :].rearrange("p a m -> p (a m)"),
                                  mout_in[l, :, pc * 2:(pc + 1) * 2, :].rearrange("p a m -> p (a m)"), "act", [t_MOUT])
                    for pc in range(4):
                        stg4.load(WO[:, pc * 2:(pc + 1) * 2, :].rearrange("p a m -> p (a m)"),
                                  wo_in[l, :, pc * 2:(pc + 1) * 2, :].rearrange("p a m -> p (a m)"), "dve" if pc % 2 == 0 else "act", [t_WO])
                    GAg = [sb(f"GAg{i}", [128, 512], scope=ph) for i in range(2)]; t_GAg = toks(2)
                    GBg = [sb(f"GBg{i}", [128, 512], scope=ph) for i in range(2)]; t_GBg = toks(2)
                    T1m = [sb(f"T1m{i}", [128, 512], scope=ph) for i in range(2)]; t_T1m = toks(2)
                    T2m = [sb(f"T2m{i}", [128, 512], scope=ph) for i in range(2)]; t_T2m = toks(2)
                    MG = sb("MG", [128, 8, 512], BF16, scope=ph); t_MG = toks(8)
                    psA = [ps(f"psA{i}", [128, 512], scope=ph) for i in range(2)]; t_psA = toks(2)
                    psB = [ps(f"psB{i}", [128, 512], scope=ph) for i in range(2)]; t_psB = toks(2)
                    psC = [ps(f"psC{i}", [128, 512], scope=ph) for i in range(2)]; t_psC = toks(2)
                    gcol = mods[:, l * 2, 16:24]
                    for tb in range(4):
                        cs = slice(tb * 512, (tb + 1) * 512)
                        YA = YAs[tb % 2]; t_YA = t_YAs[tb % 2]
                        OTb = OTbs[tb % 2]; t_OTb = t_OTbs[tb % 2]
                        cx.dma("sp", YA[:], yaT_d[l][:, cs].rearrange("(h n) t -> n h t", n=64), r=[t_yad], w=[t_YA])
                        cx.dma("sp", OTb[:], oT_d[l][:, cs].rearrange("(h n) t -> n h t", n=64), r=[t_od], w=[t_OTb])
                        for mc in range(8):
                            i = mc % 2
                            cx.dma("sp", GAg[i][:], projT[l][(19 + mc) * 128:(20 + mc) * 128, cs], w=[t_GAg[i]])
                            cx.dma("sp", GBg[i][:], projT[l][(27 + mc) * 128:(28 + mc) * 128, cs], w=[t_GBg[i]])
                            for h in range(8):
                                mm(cx, psA[i][:], ROUT[:, h, mc * 128:(mc + 1) * 128], YA[:, h, :], h == 0, h == 7, r=[t_ROUT, t_YA], w=[t_psA[i]])
                            for h in range(8):
                                mm(cx, psB[i][:], MOUT[:, h, mc * 128:(mc + 1) * 128], OTb[:, h, :], h == 0, h == 7, r=[t_MOUT, t_OTb], w=[t_psB[i]])
                            tt(cx, "dve", T1m[i][:], psA[i][:], GAg[i][:], ALU.mult, r=[t_psA[i], t_GAg[i]], w=[t_T1m[i]])
                            tt(cx, "dve", T2m[i][:], psB[i][:], GBg[i][:], ALU.mult, r=[t_psB[i], t_GBg[i]], w=[t_T2m[i]])
                            tt(cx, "pool", MG[:, mc, :], T1m[i][:], T2m[i][:], ALU.add, r=[t_T1m[i], t_T2m[i]], w=[t_MG[mc]])
                        for oc in range(8):
                            j = oc % 2
                            for mc in range(8):
                                mm(cx, psC[j][:], WO[:, mc, oc * 128:(oc + 1) * 128], MG[:, mc, :], mc == 0, mc == 7, r=[t_WO, t_MG[mc]], w=[t_psC[j]])
                            stt(cx, xT[:, oc, cs], psC[j][:], gcol[:, oc:oc + 1], xT[:, oc, cs], ALU.mult, ALU.add,
                                r=[t_psC[j], t_x[oc], t_mods], w=[t_x[oc]])
                    cx.barrier()
                if stop == f"M4_{l}":
                    for kc in range(8):
                        cx.dma("sp", xs_d[kc * 128:(kc + 1) * 128, :], xT[:, kc, :], r=[t_x[kc]], w=[t_xs])
                    cx.barrier()
                    break
                moe = (l % 2 == 1)
                with ExitStack() as ph:
                    hT = sb("hT2", [128, 8, S + 1], BF16, scope=ph); t_h = toks(8)
                    CT = sb("CT", [8, S], scope=ph); t_CT = Tok()
                    with ExitStack() as p2:
                        if moe:
                            h32 = sb("h32", [128, 8, 512], scope=p2); t_h32 = Tok()
                            LOG = sb("LOG", [128, 16, 8], scope=p2); t_LOG = Tok()
                            RW = sb("RW", [128, 8, 8], scope=p2); t_RW = Tok()
                            cx.dma("sp", RW[:], rw_in[:, :, :], w=[t_RW])
                            psL = ps("psL", [128, 512], scope=p2); t_psL = Tok()
                            rbo = VEC_COLS["router_b"][0]

                            def hook(tb):
                                for tsub in range(4):
                                    ti = tb * 4 + tsub
                                    for kc in range(8):
                                        mm(cx, psL[:, 0:8], h32[:, kc, tsub * 128:(tsub + 1) * 128], RW[:, kc, :], kc == 0, kc == 7,
                                           r=[t_h32, t_RW], w=[t_psL])
                                    tt(cx, "dve", LOG[:, ti, :], psL[:, 0:8], vecs[:, rbo:rbo + 8], ALU.add, r=[t_psL, t_vecs], w=[t_LOG])
                            rms_to_h(p2, xT, t_x, hT, t_h, acol[:, l * 2 + 1, :], mods[:, l * 2 + 1, 0:8], h32=h32, t_h32=t_h32, tb_hook=hook)
                            m1 = sb("m1", [128, 16], scope=p2); m2 = sb("m2", [128, 16], scope=p2)
                            eq1 = sb("eq1", [128, 16, 8], scope=p2); eq2 = sb("eq2", [128, 16, 8], scope=p2)
                            L2 = sb("L2", [128, 16, 8], scope=p2); COMB = sb("COMB", [128, 16, 8], scope=p2)
                            dd = sb("dd", [128, 16], scope=p2); p1 = sb("p1", [128, 16], scope=p2); p2_ = sb("p2_", [128, 16], scope=p2)
                            tk = Tok()
                            R_ = [t_LOG, tk]
                            cx.op("dve", lambda e: e.tensor_reduce(out=m1[:], in_=LOG[:], axis=AX.X, op=ALU.max), r=R_, w=[tk])
                            tt(cx, "dve", eq1[:], LOG[:], bc3(m1[:], 8), ALU.is_equal, r=R_, w=[tk])
                            stt(cx, L2[:], eq1[:], -1e30, LOG[:], ALU.mult, ALU.add, r=R_, w=[tk])
                            cx.op("dve", lambda e: e.tensor_reduce(out=m2[:], in_=L2[:], axis=AX.X, op=ALU.max), r=R_, w=[tk])
                            tt(cx, "dve", eq2[:], L2[:], bc3(m2[:], 8), ALU.is_equal, r=R_, w=[tk])
                            tt(cx, "dve", dd[:], m2[:], m1[:], ALU.subtract, r=R_, w=[tk])
                            act(cx, dd[:], dd[:], AF.Exp, r=R_, w=[tk])
                            ts(cx, "dve", p1[:], dd[:], 1.0, None, ALU.add, r=R_, w=[tk])
                            cx.op("dve", lambda e: e.reciprocal(out=p1[:], in_=p1[:]), r=R_, w=[tk])
                            tt(cx, "dve", p2_[:], dd[:], p1[:], ALU.mult, r=R_, w=[tk])
                            tt(cx, "dve", eq1[:], eq1[:], bc3(p1[:], 8), ALU.mult, r=R_, w=[tk])
                            tt(cx, "dve", eq2[:], eq2[:], bc3(p2_[:], 8), ALU.mult, r=R_, w=[tk])
                            tt(cx, "dve", COMB[:], eq1[:], eq2[:], ALU.add, r=R_, w=[tk])
                            for tb in range(4):
                                for tsub in range(4):
                                    ti = tb * 4 + tsub
                                    cx.op("pe", lambda e: e.transpose(out=psL[0:8, tsub * 128:(tsub + 1) * 128], in_=COMB[:, ti, :], identity=ident),
                                          r=[tk, t_consts], w=[t_psL])
                                cp(cx, "act", CT[:, tb * 512:(tb + 1) * 512], psL[0:8, :], r=[t_psL], w=[t_CT])
                            tap("t_CT", CT[:], [t_CT])
                        else:
                            rms_to_h(p2, xT, t_x, hT, t_h, acol[:, l * 2 + 1, :], mods[:, l * 2 + 1, 0:8])
                        cx.barrier()
                    NG = 5
                    stgf = Stager(ph, 3072, 2)
                    G = sb("G", [128, NG, S], BF16, scope=ph); t_G = toks(NG)
                    W13 = [sb(f"W13_{i}", [128, 2048], BF16, scope=ph) for i in range(2)]; t_W13 = toks(2)
                    NW2 = NG + 3
                    W2 = [sb(f"W2_{i}", [128, D], BF16, scope=ph) for i in range(NW2)]; t_W2 = toks(NW2)
                    CB = sb("CB", [128, S], BF16, scope=ph); t_CB = Tok()
                    SA = [sb(f"SA{i}", [128, 512], BF16, scope=ph) for i in range(2)]; t_SA = toks(2)
                    TB = [sb(f"TB{i}", [128, 512], BF16, scope=ph) for i in range(2)]; t_TB = toks(2)
                    psA = [ps(f"fpsA{i}", [128, 512], scope=ph) for i in range(2)]; t_psA = toks(2)
                    psB = [ps(f"fpsB{i}", [128, 512], scope=ph) for i in range(2)]; t_psB = toks(2)
                    psO = [ps(f"fpsO{i}", [128, 512], scope=ph) for i in range(2)]; t_psO = toks(2)
                    g2col = mods[:, l * 2 + 1, 16:24]
                    wi = 0
                    w2i = 0
                    ai = 0
                    oi = 0
                    for e in range(NEXP if moe else 1):
                        pack = moe_in[e] if moe else ffn_in
                        nf = NF_MOE if moe else NF_DENSE
                        if moe:
                            for tb in range(4):
                                cs = slice(tb * 512, (tb + 1) * 512)
                                mm(cx, psO[oi][:], sel[0:8, e, :], CT[:, cs], True, True, r=[t_sel, t_CT], w=[t_psO[oi]])
                                cp(cx, "act", CB[:, cs], psO[oi][:], r=[t_psO[oi]], w=[t_CB])
                                oi = 1 - oi
                        groups = [list(range(i, min(i + NG, nf))) for i in range(0, nf, NG)]
                        for grp in groups:
                            w2s = []
                            for fi, f in enumerate(grp):
                                w13 = W13[wi % 2]; t_w13 = t_W13[wi % 2]
                                wi += 1
                                w2 = W2[w2i % NW2]; t_w2 = t_W2[w2i % NW2]
                                w2i += 1
                                w2s.append((w2, t_w2))
                                sbi = stgf.i % 2
                                stgf.i += 1
                                cx.dma("sp", stgf.bufs[sbi][:], pack[f, :, :], w=[stgf.tk[sbi]])
                                cp(cx, "dve", w13[:], stgf.bufs[sbi][:, 0:2048], r=[stgf.tk[sbi]], w=[t_w13])
                                cp(cx, "act", w2[:], stgf.bufs[sbi][:, 2048:3072], r=[stgf.tk[sbi]], w=[t_w2])
                                for tb in range(4):
                                    cs = slice(tb * 512, (tb + 1) * 512)
                                    hs = slice(1 + tb * 512, 1 + (tb + 1) * 512)
                                    for kc in range(8):
                                        mm(cx, psA[ai][:], w13[:, kc * 128:(kc + 1) * 128], hT[:, kc, hs], kc == 0, kc == 7, r=[t_w13, t_h[kc]], w=[t_psA[ai]])
                                    for kc in range(8):
                                        mm(cx, psB[ai][:], w13[:, 1024 + kc * 128:1024 + (kc + 1) * 128], hT[:, kc, hs], kc == 0, kc == 7, r=[t_w13, t_h[kc]], w=[t_psB[ai]])
                                    act(cx, SA[ai][:], psA[ai][:], AF.Silu, r=[t_psA[ai]], w=[t_SA[ai]])
                                    if moe:
                                        tt(cx, "dve", TB[ai][:], psB[ai][:], CB[:, cs], ALU.mult, r=[t_psB[ai], t_CB], w=[t_TB[ai]])
                                        tt(cx, "pool", G[:, fi, cs], SA[ai][:], TB[ai][:], ALU.mult, r=[t_SA[ai], t_TB[ai]], w=[t_G[fi]])
                                    else:
                                        tt(cx, "dve", G[:, fi, cs], psB[ai][:], SA[ai][:], ALU.mult, r=[t_psB[ai], t_SA[ai]], w=[t_G[fi]])
                                    ai = 1 - ai
                            for mc in range(8):
                                for tb in range(4):
                                    cs = slice(tb * 512, (tb + 1) * 512)
                                    for fi in range(len(grp)):
                                        mm(cx, psO[oi][:], w2s[fi][0][:, mc * 128:(mc + 1) * 128], G[:, fi, cs], fi == 0, fi == len(grp) - 1,
                                           r=[w2s[fi][1], t_G[fi]], w=[t_psO[oi]])
                                    stt(cx, xT[:, mc, cs], psO[oi][:], g2col[:, mc:mc + 1], xT[:, mc, cs], ALU.mult, ALU.add,
                                        r=[t_psO[oi], t_x[mc], t_mods], w=[t_x[mc]])
                                    oi = 1 - oi
                    cx.barrier()
                if stop == f"F_{l}":
                    for kc in range(8):
                        cx.dma("sp", xs_d[kc * 128:(kc + 1) * 128, :], xT[:, kc, :], r=[t_x[kc]], w=[t_xs])
                    cx.barrier()
                    break
            else:
                with ExitStack() as ph:
                    sq = [sb(f"fsq{i}", [128, 512], scope=ph) for i in range(2)]; t_sq = toks(2)
                    rstd = [sb(f"frstd{i}", [128, 512], scope=ph) for i in range(2)]; t_rstd = toks(2)
                    ot = [sb(f"fot{i}", [128, 512], scope=ph) for i in range(4)]; t_ot = toks(4)
                    pss = [ps(f"fpss{i}", [128, 512], scope=ph) for i in range(2)]; t_pss = toks(2)
                    t_out = Tok()
                    k_ = 0
                    for tb in range(4):
                        cs = slice(tb * 512, (tb + 1) * 512)
                        pb_ = tb % 2
                        for kc in range(8):
                            b = k_ % 2
                            k_ += 1
                            act(cx, sq[b][:], xT[:, kc, cs], AF.Square, r=[t_x[kc]], w=[t_sq[b]])
                            mm(cx, pss[pb_][:], ones128, sq[b][:], kc == 0, kc == 7, r=[t_sq[b], t_consts], w=[t_pss[pb_]])
                        act(cx, rstd[pb_][:], pss[pb_][:], AF.Sqrt, r=[t_pss[pb_]], w=[t_rstd[pb_]], bias=NORM_EPS, scale=1.0 / D)
                        cx.op("dve", lambda e: e.reciprocal(out=rstd[pb_][:], in_=rstd[pb_][:]), r=[t_rstd[pb_]], w=[t_rstd[pb_]])
                        for kc in range(8):
                            b = k_ % 4
                            k_ += 1
                            stt(cx, ot[b][:], xT[:, kc, cs], acol[:, NL * 2, kc:kc + 1], rstd[pb_][:], ALU.mult, ALU.mult,
                                r=[t_x[kc], t_rstd[pb_], t_acol], w=[t_ot[b]])
                            cx.dma("sp", out_T[kc * 128:(kc + 1) * 128, cs], ot[b][:], r=[t_ot[b]], w=[t_out])
                    cx.barrier()
            if xT is not None:
                xscope.close()
    except StopBuild:
        pass
    return nc


def _host_layout(inputs):
    f = lambda a: np.ascontiguousarray(a, dtype=np.float32)
    w = {}
    col = lambda v: np.asarray(v, np.float32).reshape(-1, 128).T
    col64 = lambda v: np.pad(np.asarray(v, np.float32).reshape(-1, 64).T, ((0, 64), (0, 0)))
    vecs = np.zeros((128, NV), np.float32)

    def put(name, arr):
        o, n = VEC_COLS[name]
        assert arr.shape == (128, n), (name, arr.shape, n)
        vecs[:, o:o + n] = arr

    for l in range(NL):
        put(f"norm_mix{l}", col(inputs["norm_mix"][l]))
        put(f"norm_ffn{l}", col(inputs["norm_ffn"][l]))
        put(f"ada_b{l}0", col(inputs["ada_b"][l, 0]))
        put(f"ada_b{l}1", col(inputs["ada_b"][l, 1]))
        put(f"mu{l}", col(inputs["tshift_mu"][l]))
        put(f"q_norm{l}", col(inputs["q_norm"][l]))
        put(f"kv_norm{l}", col(inputs["kv_norm"][l]))
        for n in ("w0", "a0", "k_k", "k_a", "lnx_g", "lnx_b"):
            put(f"{n}{l}", col64(inputs[n][l]))
        put(f"r_k{l}", col64(inputs["r_k"][l].reshape(-1)))
    put("norm_final", col(inputs["norm_final"]))
    inv_freq = (10000.0 ** (-np.arange(0, 32, 2, dtype=np.float32) / 32)).astype(np.float32)
    rf = np.zeros((128, 1), np.float32); rs = np.zeros((128, 1), np.float32)
    rf[64:80, 0] = inv_freq; rf[80:96, 0] = inv_freq
    rs[64:80, 0] = -1.0; rs[80:96, 0] = 1.0
    put("ropef", rf); put("ropes", rs)
    put("router_b", np.tile(np.asarray(inputs["router_b"][0], np.float32)[None, :], (128, 1)))
    w["vecs"] = vecs

    consts = np.zeros((128, 5, 128), np.float32)
    consts[:, 0, :] = np.eye(128)
    consts[:, 1, :] = 1.0
    kk = np.arange(128)[:, None] // 64; qq = np.arange(128)[None, :] // 64
    consts[:, 2, :] = (kk <= qq)
    w["consts"] = consts
    c64 = np.zeros((64, 2, 8, 128), np.float32)
    s_ = np.arange(64)[:, None]; t_ = np.arange(64)[None, :]
    mT = np.concatenate([(s_ < t_), (s_ <= t_)], axis=1).astype(np.float32)
    c64[:, 0, :, :] = mT[:, None, :]
    c64[:, 1, :, 0:64] = (s_ > t_).astype(np.float32)[:, None, :]
    c64[:, 1, :, 64:128] = np.eye(64, dtype=np.float32)[:, None, :]
    w["c64"] = c64
    sel = np.zeros((16, 8, 128), np.float32)
    for e in range(8):
        sel[e, e, :] = 1.0
    w["sel"] = sel

    w["adaw"] = f(np.asarray(inputs["ada_w"]).reshape(NL, 2, 8, 128, 3072).transpose(0, 1, 3, 2, 4))
    win = np.asarray(inputs["w_in"], np.float32)
    kr = np.zeros((NL, D, 128), np.float32)
    kr[:, :, 64:96] = win[:, :, 2432:2464]
    winp = np.concatenate([win[:, :, 0:2432], win[:, :, 2464:4512], kr], axis=2)
    w["win"] = f(winp.reshape(NL, 8, 128, NCH // 4, 4, 128).transpose(0, 3, 2, 4, 1, 5))
    w["waup"] = f(np.concatenate([inputs["w_up"], inputs["a_up"]], axis=1))
    w["gup"] = f(inputs["g_up"])
    w["rout"] = f(np.asarray(inputs["rwkv_out"]).reshape(NL, 8, 64, D).transpose(0, 2, 1, 3))
    wqb = np.asarray(inputs["w_qb"], np.float32)
    w["wqb"] = f(wqb.reshape(NL, 3, 128, 768).transpose(0, 2, 1, 3))
    wq4 = wqb.reshape(NL, 384, 8, 96).copy()
    sw = wq4.copy()
    sw[..., 64:80] = wq4[..., 80:96]
    sw[..., 80:96] = wq4[..., 64:80]
    w["wqbs"] = f(sw.reshape(NL, 3, 128, 768).transpose(0, 2, 1, 3))
    wkvb = np.asarray(inputs["w_kvb"], np.float32).reshape(NL, 2, 128, 8, 128)
    w["wkvk"] = f(wkvb[..., 0:64].reshape(NL, 2, 128, 512).transpose(0, 2, 1, 3))
    w["wkvv"] = f(wkvb[..., 64:128].reshape(NL, 2, 128, 512).transpose(0, 2, 1, 3))
    w["mout"] = f(np.asarray(inputs["mla_out"]).reshape(NL, 8, 64, D).transpose(0, 2, 1, 3))
    w["wo"] = f(np.asarray(inputs["w_o"]).reshape(NL, 8, 128, D).transpose(0, 2, 1, 3))

    def pack(w1, w3, w2, nf):
        a = np.asarray(w1, np.float32).reshape(8, 128, nf, 128).transpose(2, 1, 0, 3).reshape(nf, 128, 1024)
        b = np.asarray(w3, np.float32).reshape(8, 128, nf, 128).transpose(2, 1, 0, 3).reshape(nf, 128, 1024)
        c = np.asarray(w2, np.float32).reshape(nf, 128, 1024)
        return np.concatenate([a, b, c], axis=2)

    w["ffnp"] = f(pack(inputs["ffn_w1"][0], inputs["ffn_w3"][0], inputs["ffn_w2"][0], NF_DENSE))
    w["moep"] = f(np.stack([pack(inputs["moe_w1"][0, e], inputs["moe_w3"][0, e], inputs["moe_w2"][0, e], NF_MOE)
                            for e in range(NEXP)]))
    w["routw"] = f(np.asarray(inputs["router_w"][0]).reshape(8, 128, 8).transpose(1, 0, 2))
    return w


def _core_inputs(inputs, b):
    return {
        "xT_in": np.ascontiguousarray(np.asarray(inputs["x"][b], np.float32).T),
        "cT": np.ascontiguousarray(np.asarray(inputs["c"][b], np.float32).reshape(8, 128).T),
        "pos": np.ascontiguousarray(np.tile(np.asarray(inputs["positions"][b], np.int32)[None, :], (32, 1))),
    }


def kernel(**inputs):
    w = _host_layout(inputs)
    nc = build()
    in_maps = []
    for b in range(8):
        m = dict(w)
        m.update(_core_inputs(inputs, b))
        in_maps.append(m)
    res = run_bass_kernel_spmd(nc, in_maps, core_ids=list(range(8)))
    out = np.stack([np.ascontiguousarray(r["outT"].T) for r in res.results], axis=0)
    return out.astype(np.float32)
```
